# Optimizing a Trainium2 kernel written in Bass

```python
import jax
import jax.numpy as jnp
from jax import lax
import numpy as np

D_MODEL = 2048
BATCH = 4
SEQ = 2048
DEPTH = 1

MLSTM_HEADS = 8
MLSTM_DV = D_MODEL // 16
MLSTM_DK = MLSTM_DV // 2
MLSTM_CHUNK = 64
GATE_SOFTCAP = 15.0
MLSTM_WIDTH = MLSTM_HEADS * MLSTM_DV

MOBA_HEADS = 8
MOBA_HEAD_DIM = D_MODEL // 16
MOBA_BLOCK = 256
MOBA_TOPK = 3
MOBA_QCHUNK = 64
MOBA_WIDTH = MOBA_HEADS * MOBA_HEAD_DIM

N_EXPERTS = 32
TOP_K = 4
D_EXPERT = D_MODEL
SWIGLU_ALPHA = 1.702
SWIGLU_LIMIT = 7.0

NORM_EPS = 1e-6
NEG_INF = -1e30

IN_WIDTHS = (MLSTM_HEADS * MLSTM_DK, MLSTM_HEADS * MLSTM_DK, MLSTM_WIDTH, MLSTM_WIDTH,
             MLSTM_HEADS, MLSTM_HEADS, MOBA_WIDTH, MOBA_WIDTH, MOBA_WIDTH, D_MODEL, D_MODEL)
D_IN = sum(IN_WIDTHS)

kernel_name = 'hybrid_mlstm_moba_moe_block'


def rms_norm(x, g):
    xf = x.astype(jnp.float32)
    y = xf * lax.rsqrt(jnp.mean(xf * xf, axis=-1, keepdims=True) + NORM_EPS)
    return (y * g).astype(x.dtype)


def modulate(h, shift, scale):
    return h * (1.0 + scale[:, None, :]) + shift[:, None, :]


def soft_cap(x, cap):
    return cap * jnp.tanh(x / cap)


def mlstm_chunkwise(q, k, v, log_i, log_f):
    B, H, S, DK = q.shape
    DV = v.shape[-1]
    L = MLSTM_CHUNK
    NC = S // L
    q = q.reshape(B, H, NC, L, DK)
    k = k.reshape(B, H, NC, L, DK)
    v = v.reshape(B, H, NC, L, DV)
    li = log_i.reshape(B, H, NC, L)
    lf = log_f.reshape(B, H, NC, L)
    b = jnp.cumsum(lf, axis=-1)
    g = b[..., -1]
    a = g[..., None] - b + li
    m_loc = jnp.max(a, axis=-1)
    w = jnp.exp(a - m_loc[..., None])
    C_loc = jnp.einsum('bhcl,bhcld,bhcle->bhcde', w, v, k)
    n_loc = jnp.einsum('bhcl,bhcle->bhce', w, k)

    def step(carry, inp):
        C, n, m = carry
        Cl, nl, ml, gc = inp
        m_new = jnp.maximum(gc + m, ml)
        s_prev = jnp.exp(gc + m - m_new)
        s_loc = jnp.exp(ml - m_new)
        C_new = s_prev[..., None, None] * C + s_loc[..., None, None] * Cl
        n_new = s_prev[..., None] * n + s_loc[..., None] * nl
        return (C_new, n_new, m_new), (C, n, m)

    def to_front(t):
        return jnp.moveaxis(t, 2, 0)

    init = (jnp.zeros((B, H, DV, DK), jnp.float32), jnp.zeros((B, H, DK), jnp.float32),
            jnp.zeros((B, H), jnp.float32))
    _, (C_in, n_in, m_in) = lax.scan(step, init, (to_front(C_loc), to_front(n_loc),
                                                   to_front(m_loc), to_front(g)))
    C_in = jnp.moveaxis(C_in, 0, 2)
    n_in = jnp.moveaxis(n_in, 0, 2)
    m_in = jnp.moveaxis(m_in, 0, 2)
    causal = jnp.tril(jnp.ones((L, L), dtype=bool))
    d = jnp.where(causal, b[..., :, None] - b[..., None, :] + li[..., None, :], -jnp.inf)
    inter_log = b + m_in[..., None]
    m_j = jnp.maximum(inter_log, jnp.max(d, axis=-1))
    s_intra = jnp.einsum('bhcjd,bhcsd->bhcjs', q, k) * jnp.exp(d - m_j[..., None])
    s_inter = jnp.exp(inter_log - m_j)
    num = (jnp.einsum('bhcjs,bhcsd->bhcjd', s_intra, v)
           + s_inter[..., None] * jnp.einsum('bhcde,bhcje->bhcjd', C_in, q))
    den = jnp.sum(s_intra, axis=-1) + s_inter * jnp.einsum('bhce,bhcje->bhcj', n_in, q)
    h = num / jnp.maximum(jnp.abs(den), jnp.exp(-m_j))[..., None]
    return h.reshape(B, H, S, DV)


def moba_attention(q, k, v):
    B, H, S, Dh = q.shape
    nb = -(-S // MOBA_BLOCK)
    pad = nb * MOBA_BLOCK - S
    kp = jnp.pad(k, ((0, 0), (0, 0), (0, pad), (0, 0)))
    vp = jnp.pad(v, ((0, 0), (0, 0), (0, pad), (0, 0)))
    kb = kp.reshape(B, H, nb, MOBA_BLOCK, Dh)
    vb = vp.reshape(B, H, nb, MOBA_BLOCK, Dh)
    k_mean = jnp.mean(kb.astype(jnp.float32), axis=3)
    q_blk = jnp.arange(S) // MOBA_BLOCK
    past = jnp.arange(nb)[None, :] < q_blk[:, None]
    gate = jnp.einsum('bhsd,bhnd->bhsn', q.astype(jnp.float32), k_mean)
    gate = jnp.where(past, gate, NEG_INF)
    n_sel = min(MOBA_TOPK, nb)
    _, sel = lax.top_k(gate, n_sel)
    sel_ok = sel < q_blk[:, None]
    nq = S // MOBA_QCHUNK

    def to_chunks(t):
        return jnp.moveaxis(t.reshape(B, H, nq, MOBA_QCHUNK, *t.shape[3:]), 2, 0)

    bi = jnp.arange(B)[:, None, None]
    hi = jnp.arange(H)[None, :, None]
    scale = Dh ** -0.5

    def attend_chunk(args):
        ci, qc, sc, okc = args
        q0 = ci * MOBA_QCHUNK
        blk = q0 // MOBA_BLOCK
        k_own = lax.dynamic_slice_in_dim(kp, blk * MOBA_BLOCK, MOBA_BLOCK, axis=2)
        v_own = lax.dynamic_slice_in_dim(vp, blk * MOBA_BLOCK, MOBA_BLOCK, axis=2)
        q_pos = q0 + jnp.arange(MOBA_QCHUNK)
        k_pos = blk * MOBA_BLOCK + jnp.arange(MOBA_BLOCK)
        own = jnp.einsum('bhqd,bhkd->bhqk', qc, k_own).astype(jnp.float32) * scale
        own = jnp.where(k_pos[None, :] <= q_pos[:, None], own, NEG_INF)
        logits = []
        for j in range(n_sel):
            kj = kb[bi, hi, sc[..., j]]
            lj = jnp.einsum('bhqd,bhqkd->bhqk', qc, kj).astype(jnp.float32) * scale
            logits.append(jnp.where(okc[..., j, None], lj, NEG_INF))
        logits.append(own)
        p = jax.nn.softmax(jnp.concatenate(logits, axis=-1), axis=-1)
        out = jnp.einsum('bhqk,bhkd->bhqd', p[..., n_sel * MOBA_BLOCK:], v_own.astype(jnp.float32))
        for j in range(n_sel):
            vj = vb[bi, hi, sc[..., j]]
            out = out + jnp.einsum('bhqk,bhqkd->bhqd',
                                   p[..., j * MOBA_BLOCK:(j + 1) * MOBA_BLOCK],
                                   vj.astype(jnp.float32))
        return out.astype(q.dtype)

    out = lax.map(attend_chunk, (jnp.arange(nq), to_chunks(q), to_chunks(sel), to_chunks(sel_ok)))
    return jnp.moveaxis(out, 0, 2).reshape(B, H, S, Dh)


def expert_ffn(t, w_up, b_up, w_down, b_down):
    hh = t @ w_up + b_up
    glu, lin = jnp.split(hh, 2, axis=-1)
    glu = jnp.minimum(glu, SWIGLU_LIMIT)
    lin = jnp.clip(lin, -SWIGLU_LIMIT, SWIGLU_LIMIT)
    act = glu * jax.nn.sigmoid(SWIGLU_ALPHA * glu) * (lin + 1.0)
    return act @ w_down + b_down


def setup_inputs(seed: int = 0) -> dict:
    key = jax.random.key(seed)
    ks = jax.random.split(key, 24)
    L = DEPTH
    D = D_MODEL

    def nrm(k, shape, scale):
        return jax.random.normal(k, shape, jnp.float32) * scale

    return {
        'x': nrm(ks[0], (BATCH, SEQ, D), 1.0),
        'c': nrm(ks[1], (BATCH, D), 1.0),
        'w_ada': nrm(ks[2], (L, D, 6 * D), 0.5 * D ** -0.5),
        'b_ada': nrm(ks[3], (L, 6 * D), 0.02),
        'g_mix': 1.0 + nrm(ks[4], (L, D), 0.02),
        'w_in': nrm(ks[5], (L, D, D_IN), D ** -0.5),
        'b_igate': nrm(ks[6], (L, MLSTM_HEADS), 0.1),
        'b_fgate': jnp.linspace(3.0, 6.0, MLSTM_HEADS, dtype=jnp.float32)[None, :]
                   + nrm(ks[7], (L, MLSTM_HEADS), 0.1),
        'g_mlstm_out': 1.0 + nrm(ks[8], (L, MLSTM_HEADS, MLSTM_DV), 0.02),
        'g_q': 1.0 + nrm(ks[9], (L, MOBA_HEAD_DIM), 0.02),
        'g_k': 1.0 + nrm(ks[10], (L, MOBA_HEAD_DIM), 0.02),
        'w_branch_a': nrm(ks[11], (L, MLSTM_WIDTH, D), MLSTM_WIDTH ** -0.5),
        'w_branch_b': nrm(ks[12], (L, MOBA_WIDTH, D), MOBA_WIDTH ** -0.5),
        'w_out': nrm(ks[13], (L, D, D), D ** -0.5),
        'g_ffn': 1.0 + nrm(ks[14], (L, D), 0.02),
        'w_router': nrm(ks[15], (L, D, N_EXPERTS), D ** -0.5),
        'b_router': nrm(ks[16], (L, N_EXPERTS), 0.01),
        'w_up': nrm(ks[17], (L, N_EXPERTS, D, 2 * D_EXPERT), D ** -0.5),
        'b_up': nrm(ks[18], (L, N_EXPERTS, 2 * D_EXPERT), 0.02),
        'w_down': nrm(ks[19], (L, N_EXPERTS, D_EXPERT, D), D_EXPERT ** -0.5),
        'b_down': nrm(ks[20], (L, N_EXPERTS, D), 0.02),
    }


def reference(x, c, w_ada, b_ada, g_mix, w_in, b_igate, b_fgate, g_mlstm_out, g_q, g_k,
              w_branch_a, w_branch_b, w_out, g_ffn, w_router, b_router, w_up, b_up,
              w_down, b_down):
    B, S, D = x.shape
    offsets = []
    acc = 0
    for wdt in IN_WIDTHS[:-1]:
        acc += wdt
        offsets.append(acc)
    for l in range(DEPTH):
        mod = jax.nn.silu(c) @ w_ada[l] + b_ada[l]
        sh1, sc1, gt1, sh2, sc2, gt2 = jnp.split(mod, 6, axis=-1)

        h = modulate(rms_norm(x, g_mix[l]), sh1, sc1)
        z = h @ w_in[l]
        qa, ka, va, oa, ia, fa, qb, kb, vb, ga, gb = jnp.split(z, offsets, axis=-1)

        q_m = qa.reshape(B, S, MLSTM_HEADS, MLSTM_DK).transpose(0, 2, 1, 3).astype(jnp.float32) * (MLSTM_DK ** -0.5)
        k_m = ka.reshape(B, S, MLSTM_HEADS, MLSTM_DK).transpose(0, 2, 1, 3).astype(jnp.float32)
        v_m = va.reshape(B, S, MLSTM_HEADS, MLSTM_DV).transpose(0, 2, 1, 3).astype(jnp.float32)
        log_i = soft_cap((ia + b_igate[l]).astype(jnp.float32), GATE_SOFTCAP).transpose(0, 2, 1)
        log_f = jax.nn.log_sigmoid(soft_cap((fa + b_fgate[l]).astype(jnp.float32), GATE_SOFTCAP)).transpose(0, 2, 1)
        h_m = mlstm_chunkwise(q_m, k_m, v_m, log_i, log_f).transpose(0, 2, 1, 3)
        h_m = rms_norm(h_m, g_mlstm_out[l]).astype(x.dtype)
        h_m = h_m * jax.nn.sigmoid(oa.reshape(B, S, MLSTM_HEADS, MLSTM_DV))
        y_a = h_m.reshape(B, S, MLSTM_WIDTH) @ w_branch_a[l]

        q_b = rms_norm(qb.reshape(B, S, MOBA_HEADS, MOBA_HEAD_DIM), g_q[l]).transpose(0, 2, 1, 3)
        k_b = rms_norm(kb.reshape(B, S, MOBA_HEADS, MOBA_HEAD_DIM), g_k[l]).transpose(0, 2, 1, 3)
        v_b = vb.reshape(B, S, MOBA_HEADS, MOBA_HEAD_DIM).transpose(0, 2, 1, 3)
        o_b = moba_attention(q_b, k_b, v_b).transpose(0, 2, 1, 3).reshape(B, S, MOBA_WIDTH)
        y_b = o_b.astype(x.dtype) @ w_branch_b[l]

        y = jax.nn.sigmoid(ga) * y_a + jax.nn.sigmoid(gb) * y_b
        x = x + gt1[:, None, :] * (y @ w_out[l])

        t = modulate(rms_norm(x, g_ffn[l]), sh2, sc2).reshape(B * S, D)
        logits = (t @ w_router[l] + b_router[l]).astype(jnp.float32)
        top_val, top_idx = lax.top_k(logits, TOP_K)
        top_w = jax.nn.softmax(top_val, axis=-1)
        combine = jnp.sum(jax.nn.one_hot(top_idx, N_EXPERTS, dtype=jnp.float32) * top_w[..., None],
                          axis=1).astype(t.dtype)
        moe = jnp.zeros_like(t)
        for e in range(N_EXPERTS):
            out_e = expert_ffn(t, w_up[l, e], b_up[l, e], w_down[l, e], b_down[l, e])
            moe = moe + combine[:, e:e + 1] * out_e.astype(t.dtype)
        x = x + gt2[:, None, :] * moe.reshape(B, S, D)
    return x
```

```python
import numpy as np
from contextlib import ExitStack
import concourse.bass as bass
import concourse.mybir as mybir
from concourse.bass_utils import run_bass_kernel_spmd

F32 = mybir.dt.float32
BF16 = mybir.dt.bfloat16
AF = mybir.ActivationFunctionType
ALU = mybir.AluOpType
AX = mybir.AxisListType

D = 2048
KC = 16
NEXP = 32
EPS = 1e-6

O_QA, O_KA, O_VA, O_OA, O_IA, O_FA = 0, 512, 1024, 2048, 3072, 3080
O_QB, O_KB, O_VB, O_GA, O_GB = 3088, 4112, 5136, 6160, 8208
D_IN = 10256

C_ID, C_TRI, C_ONES, C_S127, C_MASK = 0, 128, 256, 384, 512
C_CCOL, C_GMIX, C_GFFN, C_BIF, C_GQ, C_GK, C_FLAG = 640, 656, 672, 688, 704, 705, 706
C_GMASK, C_VALID, C_BROUT = 708, 772, 836
C_HM0, C_HM1 = 868, 869
C_TOT = 870


class Prog:
    ENGS = ('pe', 'act', 'dve', 'pool', 'sp')

    def __init__(self, nc, es):
        self.nc = nc
        self.es = es
        self.q = {e: [] for e in self.ENGS}
        self.cnt = {}
        self.sems = {}
        self.last_w = {}
        self.readers = {}
        self.seen = {e: {} for e in self.ENGS}
        for e in self.ENGS:
            self._sem(e)

    def _sem(self, name):
        if name not in self.sems:
            self.sems[name] = self.es.enter_context(self.nc.semaphore(name))
            self.cnt[name] = 0
        return self.sems[name]

    def _deps(self, eng, reads, writes):
        deps = {}

        def add(ev):
            if ev is None:
                return
            s, v = ev
            if deps.get(s, 0) < v:
                deps[s] = v
        for k in reads:
            add(self.last_w.get(k))
        for k in writes:
            add(self.last_w.get(k))
            for s, v in self.readers.get(k, {}).items():
                add((s, v))
        waits = []
        for s, v in deps.items():
            if s == 'pe' and eng == 'pe':
                continue
            if self.seen[eng].get(s, 0) >= v:
                continue
            self.seen[eng][s] = v
            waits.append((s, v))
        return waits

    def _record(self, ev, reads, writes):
        for k in reads:
            d = self.readers.setdefault(k, {})
            if d.get(ev[0], 0) < ev[1]:
                d[ev[0]] = ev[1]
        for k in writes:
            self.last_w[k] = ev
            self.readers[k] = {}

    def op(self, eng, fn, reads=(), writes=()):
        waits = self._deps(eng, reads, writes)
        self.cnt[eng] += 1
        ev = (eng, self.cnt[eng])
        self._record(ev, reads, writes)
        self.q[eng].append((waits, fn, (eng, 1)))

    def dma(self, queue, fn, reads, writes, slot):
        sem = 'd_' + slot
        self._sem(sem)
        waits = self._deps(queue, reads, writes)
        prev = self.cnt[sem]
        if prev > 0 and self.seen[queue].get(sem, 0) < prev:
            self.seen[queue][sem] = prev
            waits.append((sem, prev))
        self.cnt[sem] += 16
        ev = (sem, self.cnt[sem])
        self._record(ev, reads, writes)
        self.q[queue].append((waits, fn, (sem, 16)))

    def barrier(self):
        allv = dict(self.cnt)
        for e in self.ENGS:
            waits = []
            for s, v in allv.items():
                if v == 0 or s == e:
                    continue
                if self.seen[e].get(s, 0) >= v:
                    continue
                self.seen[e][s] = v
                waits.append((s, v))
            if waits:
                self.q[e].append((waits, None, None))

    def finish(self):
        waits = [(s, v) for s, v in self.cnt.items() if v > 0 and s != 'sp']
        self.q['sp'].append((waits, None, None))

    def emit(self):
        nc = self.nc
        sems = self.sems

        def replay(name, eng):
            for waits, fn, inc in self.q[name]:
                for s, v in waits:
                    eng.wait_ge(sems[s], v)
                if fn is None:
                    continue
                ins = fn(eng)
                ins.then_inc(sems[inc[0]], inc[1])
        with nc.Block() as block:
            @block.tensor
            def _(eng):
                replay('pe', eng)

            @block.scalar
            def _(eng):
                replay('act', eng)

            @block.vector
            def _(eng):
                replay('dve', eng)

            @block.gpsimd
            def _(eng):
                replay('pool', eng)

            @block.sync
            def _(eng):
                replay('sp', eng)

    def mm(self, out, pairs, reads, writes):
        pairs = list(pairs)

        def fn(e):
            n = len(pairs)
            ins = None
            for i, (l, r) in enumerate(pairs):
                ins = e.matmul(out, lhsT=l, rhs=r, start=(i == 0), stop=(i == n - 1))
            return ins
        self.op('pe', fn, reads, writes)

    def mms(self, groups, reads, writes):
        groups = [(o, list(p)) for o, p in groups]

        def fn(e):
            ins = None
            for o, pairs in groups:
                n = len(pairs)
                for i, (l, r) in enumerate(pairs):
                    ins = e.matmul(o, lhsT=l, rhs=r, start=(i == 0), stop=(i == n - 1))
            return ins
        self.op('pe', fn, reads, writes)

    def transposes(self, items, ident, reads, writes):
        items = list(items)

        def fn(e):
            ins = None
            for o, i in items:
                ins = e.transpose(out=o, in_=i, identity=ident)
            return ins
        self.op('pe', fn, reads, writes)

    def act(self, out, in_, func, reads, writes, **kw):
        self.op('act', lambda e: e.activation(out=out, in_=in_, func=func, **kw), reads, writes)

    def ts(self, eng, out, in0, s1, s2, op0, op1, reads, writes, **kw):
        if op1 is None:
            self.op(eng, lambda e: e.tensor_scalar(out=out, in0=in0, scalar1=s1, scalar2=None, op0=op0, **kw),
                    reads, writes)
        else:
            self.op(eng, lambda e: e.tensor_scalar(out=out, in0=in0, scalar1=s1, scalar2=s2, op0=op0, op1=op1, **kw),
                    reads, writes)

    def tt(self, eng, out, in0, in1, op, reads, writes):
        self.op(eng, lambda e: e.tensor_tensor(out=out, in0=in0, in1=in1, op=op), reads, writes)

    def stt(self, eng, out, in0, scalar, in1, op0, op1, reads, writes):
        self.op(eng, lambda e: e.scalar_tensor_tensor(out=out, in0=in0, scalar=scalar, in1=in1, op0=op0, op1=op1),
                reads, writes)

    def copy(self, eng, out, in_, reads, writes):
        if eng == 'act':
            self.op('act', lambda e: e.copy(out=out, in_=in_), reads, writes)
        else:
            self.op(eng, lambda e: e.tensor_copy(out=out, in_=in_), reads, writes)

    def recip(self, out, in_, reads, writes):
        self.op('dve', lambda e: e.reciprocal(out=out, in_=in_), reads, writes)

    def reduce(self, out, in_, op, reads, writes):
        self.op('dve', lambda e: e.tensor_reduce(out=out, in_=in_, axis=AX.X, op=op), reads, writes)

    def memset(self, eng, ap, val, writes):
        self.op(eng, lambda e: e.memset(ap, val), [], writes)

    def load(self, queue, out, in_, writes, slot, reads=()):
        self.dma(queue, lambda e: e.dma_start(out=out, in_=in_), list(reads), writes, slot)


def build_program(stage=99, sub=99):
    holder = {}
    try:
        holder['nc'] = _build_program(holder, stage, sub)
    except AssertionError as e:
        if 'non-stack order' not in str(e) or 'nc' not in holder:
            raise
    return holder['nc']


def _build_program(holder, stage=99, sub=99):
    nc = bass.Bass("TRN2", target_bir_lowering=False)

    def dr(name, shape, dt=F32, kind="ExternalInput"):
        return nc.dram_tensor(name, shape, dt, kind=kind).ap()

    d_xown = dr("x_own", [1024, D])
    d_xctx = dr("x_ctx", [1024, D])
    d_cpack = dr("cpack", [128, C_TOT])
    d_gml = dr("gml_bc", [128, 1024])
    d_wada = dr("w_ada", [D, 6 * D])
    d_bada = dr("b_ada", [1, 6 * D])
    d_win = dr("w_in", [D, D_IN])
    d_wa = dr("w_branch_a", [1024, D])
    d_wb = dr("w_branch_b", [1024, D])
    d_wout = dr("w_out", [D, D])
    d_wr = dr("w_router", [D, NEXP])
    if stage >= 5:
        d_wup = dr("w_up", [NEXP, D, 2 * D])
        d_bup = dr("b_up_col", [128, NEXP * 32])
        d_wdn = dr("w_down", [NEXP, D, D])
        d_bdn = dr("b_down", [NEXP, D])
    d_out = dr("out", [1024, D], kind="ExternalOutput")
    dbg = None
    if stage < 99:
        dbg = dr("dbg", [128, 16384], kind="ExternalOutput")

    win_v = d_win.rearrange("(k p) n -> p k n", p=128)

    with ExitStack() as es:
        P = Prog(nc, es)

        def sbuf(scope, name, shape, dt):
            return scope.enter_context(nc.sbuf_tensor(name, shape, dt))

        ps = [es.enter_context(nc.psum_tensor("ps%d" % i, [128, 512], F32)) for i in range(8)]

        def PK(b):
            return [('ps', b, j) for j in range(4)]

        cp = sbuf(es, "cp", [128, C_TOT], F32)
        identb = sbuf(es, "identb", [128, 128], BF16)
        maskb = sbuf(es, "maskb", [128, 128], BF16)
        modc = sbuf(es, "modc", [128, 96], F32)
        s1c = sbuf(es, "s1c", [128, 16], F32)
        s2c = sbuf(es, "s2c", [128, 16], F32)
        gt2 = sbuf(es, "gt2", [128, D], F32)
        gqs = sbuf(es, "gqs", [128, 1], F32)
        small = sbuf(es, "small", [128, 64], F32)
        comb = sbuf(es, "comb", [128, 8, NEXP], F32)
        big64 = sbuf(es, "big64", [128, 32768], BF16)
        smix = ExitStack()
        es.callback(smix.close)
        gt1 = sbuf(smix, "gt1", [128, D], F32)

        ident = cp[:, C_ID:C_ID + 128]
        tri = cp[:, C_TRI:C_TRI + 128]
        ones = cp[:, C_ONES:C_ONES + 128]
        s127 = cp[:, C_S127:C_S127 + 128]
        mask01 = cp[:, C_MASK:C_MASK + 128]
        flag = cp[:, C_FLAG:C_FLAG + 1]

        P.load('sp', cp[:], d_cpack, ['cp'], 'cp')
        P.copy('dve', identb[:], ident, ['cp'], ['identb'])
        P.copy('dve', maskb[:], mask01, ['cp'], ['maskb'])
        P.ts('dve', gqs[:], cp[:, C_GQ:C_GQ + 1], float(128 ** -0.5), None, ALU.mult, None, ['cp'], ['gqs'])

        def rstd_from_ssq(ssq_ap, out_ap, n, inv_n, key_in, key_out):
            tmp = small[:, 0:n]
            P.ts('dve', tmp, ssq_ap, float(inv_n), float(EPS), ALU.mult, ALU.add, [key_in], ['small'])
            P.act(tmp, tmp, AF.Sqrt, ['small'], ['small'])
            P.recip(out_ap, tmp, ['small'], [key_out])

        with ExitStack() as s0:
            wada = [sbuf(s0, "wada%d" % i, [128, KC, 256], F32) for i in range(2)]
            brow = [sbuf(s0, "brow%d" % i, [1, 256], F32) for i in range(2)]
            mrow = sbuf(s0, "mrow", [1, 6 * D], F32)
            scol = sbuf(s0, "scol", [128, 16], F32)
            P.act(scol[:], cp[:, C_CCOL:C_CCOL + 16], AF.Silu, ['cp'], ['scol'])
            wada_v = d_wada.rearrange("(k p) n -> p k n", p=128)
            for n in range(48):
                sl = n % 2
                P.load('sp', wada[sl][:], wada_v[:, :, n * 256:(n + 1) * 256], [('wada', sl)], 'wada%d' % sl)
                P.load('sp', brow[sl][:], d_bada[:, n * 256:(n + 1) * 256], [('brow', sl)], 'brow%d' % sl)
                b = n % 2
                P.mm(ps[b][0:1, 0:256], [(scol[:, k:k + 1], wada[sl][:, k, :]) for k in range(KC)],
                     ['scol', ('wada', sl)], PK(b))
                P.tt('dve', mrow[0:1, n * 256:(n + 1) * 256], ps[b][0:1, 0:256], brow[sl][0:1, :],
                     ALU.add, PK(b) + [('brow', sl)], [('mrow', n)])
            mrow_keys = [('mrow', n) for n in range(48)]
            P.mms([(ps[2][:, j:j + 1], [(mrow[0:1, j * 128:(j + 1) * 128], ones[0:1, 0:1])]) for j in range(96)],
                  mrow_keys + ['cp'], PK(2))
            P.copy('dve', modc[:], ps[2][:, 0:96], PK(2), ['modc'])
            for i in range(4):
                for (gt, off, nm) in ((gt1, 2 * D, 'gt1'), (gt2, 5 * D, 'gt2')):
                    b = 3 + (i % 2) * 2 + (0 if nm == 'gt1' else 1)
                    P.mm(ps[b][:], [(ones[0:1, 0:128], mrow[0:1, off + i * 512: off + (i + 1) * 512])],
                         mrow_keys + ['cp'], PK(b))
                    P.copy('act', gt[:, i * 512:(i + 1) * 512], ps[b][:], PK(b), [(nm, i)])
            P.stt('dve', s1c[:], modc[:, 16:32], 1.0, cp[:, C_GMIX:C_GMIX + 16], ALU.add, ALU.mult,
                  ['modc', 'cp'], ['s1c'])
            P.stt('dve', s2c[:], modc[:, 64:80], 1.0, cp[:, C_GFFN:C_GFFN + 16], ALU.add, ALU.mult,
                  ['modc', 'cp'], ['s2c'])
            if stage == 0 and sub != 99:
                for i_ in range(int(sub)):
                    P.mm(ps[7][:, 0:128], [(identb[:], identb[:])], ['identb'], PK(7))
            if stage == 0:
                P.load('sp', dbg[:, 0:96], modc[:], [], 'dbg', reads=['modc'])
                P.load('sp', dbg[:, 96:112], s1c[:], [], 'dbg', reads=['s1c'])
                P.load('sp', dbg[:, 2048:4096], gt1[:], [], 'dbg', reads=[('gt1', i) for i in range(4)])
                P.load('sp', dbg[:, 4096:6144], gt2[:], [], 'dbg', reads=[('gt2', i) for i in range(4)])
            P.barrier()
        gt1k = [('gt1', i) for i in range(4)]
        gt2k = [('gt2', i) for i in range(4)]
        if stage == 0:
            P.finish()
            P.emit()
            holder['nc'] = nc
            return nc

        hT = big64[:].rearrange("p (k t) -> p k t", k=KC)
        x1 = big64[:].bitcast(F32).rearrange("p (t d) -> p t d", t=8)
        hmT = sbuf(smix, "hmT", [128, 8, 1024], BF16)
        ssq = sbuf(smix, "ssq", [128, 16], F32)
        rstd = sbuf(smix, "rstd", [128, 16], F32)

        with ExitStack() as s1:
            xt = [sbuf(s1, "xt%d" % i, [128, D], F32) for i in range(8)]
            junk = sbuf(s1, "junk", [128, D], F32)
            for tg in range(4):
                for i in range(4):
                    ti = tg * 4 + i
                    sl = ti % 8
                    src = d_xctx if ti < 8 else d_xown
                    r0 = (ti % 8) * 128
                    P.load('sp', xt[sl][:], src[r0:r0 + 128, :], [('xt', sl)], 'xt%d' % sl)
                    P.act(junk[:], xt[sl][:], AF.Square, [('xt', sl)], ['junk', ('ssq', ti)],
                          accum_out=ssq[:, ti:ti + 1])
                    rstd_from_ssq(ssq[:, ti:ti + 1], rstd[:, ti:ti + 1], 1, 1.0 / D, ('ssq', ti), ('rstd', ti))
                    P.ts('dve', xt[sl][:], xt[sl][:], rstd[:, ti:ti + 1], None, ALU.mult, None,
                         [('xt', sl), ('rstd', ti)], [('xt', sl)])
                for k in range(KC):
                    b = k % 2
                    P.transposes([(ps[b][:, i * 128:(i + 1) * 128], xt[(tg * 4 + i) % 8][:, k * 128:(k + 1) * 128])
                                  for i in range(4)], ident,
                                 [('xt', (tg * 4 + i) % 8) for i in range(4)] + ['cp'], PK(b))
                    P.act(hT[:, k, tg * 512:(tg + 1) * 512], ps[b][:], AF.Identity, PK(b) + ['s1c', 'modc'],
                          [('hT', tg)], scale=s1c[:, k:k + 1], bias=modc[:, k:k + 1])
            if stage == 1 and sub != 99:
                for i_ in range(int(sub)):
                    P.mm(ps[3][:, 0:128], [(identb[:], identb[:])], ['identb'], PK(3))
            if stage == 1:
                for k in range(KC):
                    P.load('sp', dbg[:, k * 1024:(k + 1) * 1024].bitcast(BF16), hT[:, k, :], [], 'dbg',
                           reads=[('hT', t) for t in range(4)])
            P.barrier()
        hTk = [('hT', t) for t in range(4)]
        if stage == 1:
            P.finish()
            P.emit()
            holder['nc'] = nc
            return nc


        sm = sbuf(smix, "sm", [128, 16], F32)
        junk128 = sbuf(smix, "junk128", [128, 136], F32)
        hmt = sbuf(smix, "hmt", [128, 128], BF16)
        PT = sbuf(smix, "PT", [128, 32, 128], BF16)
        psb2 = ps[2][:].bitcast(BF16)

        with ExitStack() as s2:
            wqk = sbuf(s2, "wqk", [128, KC, 512], BF16)
            wif = sbuf(s2, "wif", [128, KC, 16], BF16)
            qaT = sbuf(s2, "qaT", [128, 8, 1024], BF16)
            kaT = sbuf(s2, "kaT", [128, 4, 2048], BF16)
            gml = sbuf(s2, "gml", [128, 1024], F32)
            graw = sbuf(s2, "graw", [128, 16, 16], F32)
            tli = sbuf(s2, "tli", [128, 16, 8], F32)
            spf = sbuf(s2, "spf", [128, 16, 8], F32)
            Ecum = sbuf(s2, "Ecum", [128, 16, 8], F32)
            Bn = sbuf(s2, "Bn", [128, 16, 8], F32)
            nRn = sbuf(s2, "nRn", [128, 16, 8], F32)
            aa = sbuf(s2, "aa", [128, 16, 8], F32)
            U = sbuf(s2, "U", [128, 64, 16], F32)
            ff = sbuf(s2, "ff", [128, 8, 8], F32)
            wv = [sbuf(s2, "wv%d" % i, [128, KC, 128], BF16) for i in range(2)]
            wo = [sbuf(s2, "wo%d" % i, [128, KC, 128], BF16) for i in range(2)]
            vaug = [sbuf(s2, "vaug%d" % i, [128, 16, 136], BF16) for i in range(2)]
            gsig = [sbuf(s2, "gsig%d" % i, [128, 8, 128], F32) for i in range(2)]

            fl = lambda t: t[:].rearrange("p t h -> p (t h)")
            P.load('sp', gml[:], d_gml, ['gml'], 'gml')
            P.load('pool', wqk[:], win_v[:, :, 0:512], ['wqk'], 'wqk0')
            P.load('pool', wif[:], win_v[:, :, O_IA:O_IA + 16], ['wif'], 'wif')
            for i in range(2):
                P.memset('pool', vaug[i][:, :, 128:136], 1.0, [('vaug1', i)])

            P.mms([(ps[2][:, i * 16:(i + 1) * 16],
                    [(hT[:, k, i * 128:(i + 1) * 128], wif[:, k, :]) for k in range(KC)]) for i in range(16)],
                  hTk + ['wif'], PK(2))
            P.tt('dve', graw[:], ps[2][:, 0:256].rearrange("p (t g) -> p t g", g=16),
                 cp[:, C_BIF:C_BIF + 16].unsqueeze(1).to_broadcast([128, 16, 16]), ALU.add, PK(2) + ['cp'], ['graw'])
            P.act(tli[:], graw[:, :, 0:8], AF.Tanh, ['graw'], ['tli'], scale=1.0 / 15.0)
            P.act(spf[:], graw[:, :, 8:16], AF.Tanh, ['graw'], ['spf'], scale=1.0 / 15.0)
            P.act(spf[:], spf[:], AF.Exp, ['spf'], ['spf'], scale=-15.0)
            P.ts('dve', spf[:], spf[:], 1.0, None, ALU.add, None, ['spf'], ['spf'])
            P.act(spf[:], spf[:], AF.Ln, ['spf'], ['spf'])
            P.memset('dve', Ecum[:, 0, :], 0.0, ['Ecum'])
            for i in range(1, 16):
                P.tt('dve', Ecum[:, i, :], Ecum[:, i - 1, :], spf[:, i - 1, :], ALU.add, ['Ecum', 'spf'], ['Ecum'])
            P.mm(ps[3][:, 0:128], [(tri, fl(spf)), (ones, fl(Ecum))], ['cp', 'spf', 'Ecum'], PK(3))
            P.copy('dve', fl(Bn), ps[3][:, 0:128], PK(3), ['Bn'])
            P.mm(ps[3][:, 128:256], [(s127, fl(Bn))], ['cp', 'Bn'], PK(3))
            P.ts('dve', fl(nRn), ps[3][:, 128:256], -1.0, None, ALU.mult, None, PK(3), ['nRn'])
            P.stt('dve', fl(aa), fl(tli), 15.0, fl(Bn), ALU.mult, ALU.add, ['tli', 'Bn'], ['aa'])
            for h in range(8):
                for Tq in range(8):
                    P.act(U[:, h * 8 + Tq, :], aa[:, :, h], AF.Exp, ['aa', 'nRn'], ['U'],
                          bias=nRn[:, 7 + Tq, h:h + 1], scale=1.0)
            P.ts('dve', U[:, :, 0:8], U[:, :, 0:8], flag, None, ALU.mult, None, ['U', 'cp'], ['U'])
            P.tt('dve', fl(ff), fl(Bn)[:, 64:128], fl(nRn)[:, 56:120], ALU.add, ['Bn', 'nRn'], ['ff'])
            P.act(fl(ff), fl(ff), AF.Exp, ['ff'], ['ff'], scale=-1.0)

            cnt = 0
            for m in range(8):
                if m == 4:
                    P.load('pool', wqk[:], win_v[:, :, 512:1024], ['wqk'], 'wqk0')
                for g in ((2, 3) if m < 4 else (0, 1, 2, 3)):
                    b = 4 + cnt % 2
                    cnt += 1
                    mc = m % 4
                    P.mm(ps[b][:], [(wqk[:, k, mc * 128:(mc + 1) * 128], hT[:, k, g * 512:(g + 1) * 512])
                                    for k in range(KC)], ['wqk'] + hTk, PK(b))
                    if m < 4:
                        P.act(qaT[:, 2 * m, (g - 2) * 512:(g - 1) * 512], ps[b][:], AF.Identity, PK(b) + ['cp'], ['qaT'],
                              scale=cp[:, C_HM0:C_HM0 + 1])
                        P.act(qaT[:, 2 * m + 1, (g - 2) * 512:(g - 1) * 512], ps[b][:], AF.Identity, PK(b) + ['cp'], ['qaT'],
                              scale=cp[:, C_HM1:C_HM1 + 1])
                    else:
                        P.copy('dve', kaT[:, m - 4, g * 512:(g + 1) * 512], ps[b][:], PK(b), ['kaT'])

            import os as _os
            if stage == 2 and _os.environ.get('EXTRA1'):
                for i_ in range(int(_os.environ['EXTRA1'])):
                    P.mm(ps[7][:, 0:128], [(identb[:], identb[:])], ['identb'], PK(7))
            if stage == 2 and sub == 1:
                P.load('sp', dbg[:, 0:1024], U[:].rearrange("p a b -> p (a b)"), [], 'dbg', reads=['U'])
                P.load('sp', dbg[:, 1024:1088], fl(ff), [], 'dbg', reads=['ff'])
                P.load('sp', dbg[:, 1088:1216], fl(Bn), [], 'dbg', reads=['Bn'])
                P.load('sp', dbg[:, 1216:1344], fl(tli), [], 'dbg', reads=['tli'])
                P.load('sp', dbg[:, 2048:4096].bitcast(BF16).rearrange("p (a b) -> p a b", a=4), kaT[:, :, 1024:2048], [], 'dbg', reads=['kaT'])
                pass
                P.finish()
                P.emit()
                holder['nc'] = nc
                return nc
            gcnt = 0
            pcnt = 0
            for h in range(8 if sub >= 4 else 1):
                sl = h % 2
                P.load('pool', wv[sl][:], win_v[:, :, O_VA + h * 128:O_VA + (h + 1) * 128], [('wv', sl)], 'wv%d' % sl)
                P.load('pool', wo[sl][:], win_v[:, :, O_OA + h * 128:O_OA + (h + 1) * 128], [('wo', sl)], 'wo%d' % sl)
                for tq in range(4):
                    b = 4 + tq % 2
                    P.mms([(ps[b][:, i * 128:(i + 1) * 128],
                            [(hT[:, k, (tq * 4 + i) * 128:(tq * 4 + i + 1) * 128], wv[sl][:, k, :]) for k in range(KC)])
                           for i in range(4)], hTk + [('wv', sl)], PK(b))
                    P.copy('act' if tq % 2 == 0 else 'dve', vaug[sl][:, tq * 4:(tq + 1) * 4, 0:128],
                           ps[b][:].rearrange("p (t d) -> p t d", d=128), PK(b), [('vaug', sl)])
                for tq in range(2):
                    b = 4 + tq % 2
                    P.mms([(ps[b][:, i * 128:(i + 1) * 128],
                            [(hT[:, k, 1024 + (tq * 4 + i) * 128:1024 + (tq * 4 + i + 1) * 128], wo[sl][:, k, :])
                             for k in range(KC)]) for i in range(4)], hTk + [('wo', sl)], PK(b))
                    P.act(gsig[sl][:, tq * 4:(tq + 1) * 4, :], ps[b][:].rearrange("p (t d) -> p t d", d=128),
                          AF.Sigmoid, PK(b), [('gsig', sl)])
                P.tt('dve', gsig[sl][:], gsig[sl][:],
                     gml[:, h * 128:(h + 1) * 128].unsqueeze(1).to_broadcast([128, 8, 128]), ALU.mult,
                     [('gsig', sl), 'gml'], [('gsig', sl)])
                if stage == 2 and _os.environ.get('EXTRA2'):
                    for i_ in range(int(_os.environ['EXTRA2'])):
                        P.mm(ps[7][:, 0:128], [(identb[:], identb[:])], ['identb'], PK(7))
                if stage == 2 and sub == 2:
                    P.load('sp', dbg[:, 0:1024], gsig[sl][:].rearrange("p a b -> p (a b)"), [], 'dbg', reads=[('gsig', sl)])
                    P.finish()
                    P.emit()
                    holder['nc'] = nc
                    return nc
                hp, hm_ = h % 2, h // 2
                kq = slice(hp * 64, (hp + 1) * 64)
                for Tq in (range(8 if not (stage == 2 and 3 <= sub < 4) else int(round((sub - 3) * 10)) + 1) if sub not in (3.9, 3.05, 3.06) else ([1] if sub == 3.9 else [0])):
                    T = 8 + Tq
                    ntile = T + 1
                    nb = 5 + Tq % 2
                    if _os.environ.get('RESET'):
                        nb = 5
                        gcnt = 0
                        pcnt = 0
                    nd = ps[nb][:, 0:136]
                    pv_all = []
                    for g0 in (range(0, ntile, 4) if not (_os.environ.get('SKIP2') in ('att', 'stev') and Tq >= 1) else []):
                        Ss = list(range(g0, min(g0 + 4, ntile)))
                        b = gcnt % 2
                        gcnt += 1
                        P.mms([(ps[b][:, j * 128:(j + 1) * 128],
                                [(kaT[:, hm_, S * 128:(S + 1) * 128], qaT[:, h, Tq * 128:(Tq + 1) * 128])])
                               for j, S in enumerate(Ss)], ['kaT', 'qaT'], PK(b))
                        pv = []
                        for j, S in enumerate(Ss):
                            slot = pcnt % 32
                            pcnt += 1
                            ucol = U[:, h * 8 + Tq, S:S + 1]
                            src = ps[b][:, j * 128:(j + 1) * 128]
                            ev2 = _os.environ.get('EV2') if Tq >= 1 else None
                            if ev2 == 'none':
                                pass
                            elif ev2 == 'mix' and pcnt % 2 == 0:
                                P.act(PT[:, slot, :], src, AF.Identity, [('ps', b, j), 'U'], [('PT', slot)], scale=ucol)
                            elif ev2 == 'mix':
                                P.ts('dve', PT[:, slot, :], src, ucol, None, ALU.mult, None,
                                     [('ps', b, j), 'U'], [('PT', slot)])
                            elif ev2 == 'act':
                                P.act(PT[:, slot, :], src, AF.Identity, [('ps', b, j), 'U'], [('PT', slot)], scale=ucol)
                            elif ev2 == 'dve':
                                P.ts('dve', PT[:, slot, :], src, ucol, None, ALU.mult, None,
                                     [('ps', b, j), 'U'], [('PT', slot)])
                            else:
                                P.ts('dve', PT[:, slot, :], src, ucol, None, ALU.mult, None,
                                     [('ps', b, j), 'U'], [('PT', slot)])
                                if S == T:
                                    P.tt('dve', PT[:, slot, :], PT[:, slot, :], maskb[:], ALU.mult,
                                         [('PT', slot), 'maskb'], [('PT', slot)])
                            pv.append((slot, S))

                        if stage == 2 and sub == 2.3:
                            P.load('sp', dbg[:, 0:512].bitcast(BF16), PT[:].rearrange("p a b -> p (a b)"), [], 'dbg',
                                   reads=[('PT', i_) for i_ in range(16)])
                            P.finish()
                            P.emit()
                            holder['nc'] = nc
                            return nc

                        pv_all.extend(pv)

                    def pvfn(e, pv=list(pv_all), nd=nd, sl=sl, ntile=ntile):
                        ins = None
                        for slot, S in pv:
                            ins = e.matmul(nd, lhsT=PT[:, slot, :], rhs=vaug[sl][:, S, :],
                                           start=(S == 0), stop=(S == ntile - 1))
                        return ins
                    if _os.environ.get('SKIP2') == 'stev' and Tq >= 1:
                        pv_all = [(S_, S_) for S_ in range(9)]

                        def pvfn(e, pv=list(pv_all), nd=nd, sl=sl, ntile=9):
                            ins = None
                            for slot, S in pv:
                                ins = e.matmul(nd, lhsT=PT[:, slot, :], rhs=vaug[sl][:, S, :], start=(S == 0), stop=(S == ntile - 1))
                            return ins
                    if pv_all and not (_os.environ.get('SKIP2') == 'pv' and Tq >= 1):
                        P.op('pe', pvfn, [('PT', s_) for s_, _ in pv_all] + [('vaug', sl), ('vaug1', sl)], PK(nb))
                    if stage == 2 and _os.environ.get('EXTRA3'):
                        for i_ in range(int(_os.environ['EXTRA3'])):
                            P.mm(ps[7][:, 0:128], [(identb[:], identb[:])], ['identb'], PK(7))
                    if stage == 2 and sub == 2.5:
                        P.copy('dve', junk128[:, 0:130], nd[:, 0:130], PK(nb), ['junk128'])
                        P.load('sp', dbg[:, 0:130], junk128[:, 0:130], [], 'dbg', reads=['junk128'])
                        P.finish()
                        P.emit()
                        holder['nc'] = nc
                        return nc
                    if _os.environ.get('SKIP2') in ('epi', 'pv', 'stev') and Tq >= 1:
                        continue
                    fcol = ff[:, Tq, h:h + 1]
                    P.tt('dve', sm[:, 0:1], nd[:, 128:129], fcol, ALU.mult, PK(nb) + ['ff'], ['sm'])
                    P.stt('dve', sm[:, 1:2], sm[:, 0:1], -1.0, sm[:, 0:1], ALU.mult, ALU.max, ['sm'], ['sm'])
                    P.ts('dve', sm[:, 1:2], sm[:, 1:2], 1.0, None, ALU.max, None, ['sm'], ['sm'])
                    P.recip(sm[:, 2:3], sm[:, 1:2], ['sm'], ['sm'])
                    P.tt('dve', sm[:, 3:4], sm[:, 2:3], fcol, ALU.mult, ['sm', 'ff'], ['sm'])
                    if stage == 2 and _os.environ.get('EXTRA6'):
                        for i_ in range(int(_os.environ['EXTRA6'])):
                            P.mm(ps[7][:, 0:128], [(identb[:], identb[:])], ['identb'], PK(7))
                    if stage == 2 and sub == 2.6:
                        P.load('sp', dbg[:, 0:16], sm[:], [], 'dbg', reads=['sm'])
                        P.finish()
                        P.emit()
                        holder['nc'] = nc
                        return nc
                    P.act(junk128[:, 0:128], nd[:, 0:128], AF.Square, PK(nb) + ['sm'], ['junk128', 'sm'],
                          scale=sm[:, 3:4], accum_out=sm[:, 4:5])
                    rstd_from_ssq(sm[:, 4:5], sm[:, 5:6], 1, 1.0 / 128, 'sm', 'sm')
                    P.tt('dve', sm[:, 6:7], sm[:, 5:6], sm[:, 3:4], ALU.mult, ['sm'], ['sm'])
                    P.stt('dve', hmt[:], nd[:, 0:128], sm[:, 6:7], gsig[sl][:, Tq, :], ALU.mult, ALU.mult,
                          PK(nb) + ['sm', ('gsig', sl)], ['hmt'])
                    if stage == 2 and _os.environ.get('EXTRA7'):
                        for i_ in range(int(_os.environ['EXTRA7'])):
                            P.mm(ps[7][:, 0:128], [(identb[:], identb[:])], ['identb'], PK(7))
                    if stage == 2 and sub == 2.7:
                        P.load('sp', dbg[:, 0:64].bitcast(BF16), hmt[:], [], 'dbg', reads=['hmt'])
                        P.finish()
                        P.emit()
                        holder['nc'] = nc
                        return nc
                    P.mm(ps[2][:, 0:128], [(hmt[:], identb[:])], ['hmt', 'identb'], PK(2))
                    P.copy('act', hmT[:, h, Tq * 128:(Tq + 1) * 128], ps[2][:, 0:128], PK(2), ['hmT'])
                    P.barrier()
            if stage == 2 and _os.environ.get('EXTRA9'):
                n_, l_, b_ = _os.environ['EXTRA9'].split(',')
                for i_ in range(int(n_)):
                    P.mm(ps[int(b_)][:, 0:128], [((hmt if l_ == 'hmt' else identb)[:], identb[:])], ['hmt', 'identb'], PK(int(b_)))
            if stage == 2 and _os.environ.get('EXTRA8'):
                for i_ in range(int(_os.environ['EXTRA8'])):
                    P.mm(ps[7][:, 0:128], [(identb[:], identb[:])], ['identb'], PK(7))
            if stage == 2 and sub == 3.06:
                for _ in range(40):
                    P.mm(ps[3][:, 0:128], [(hmt[:], identb[:])], ['hmt', 'identb'], PK(3))
            if stage == 2 and sub == 3.05:
                for _ in range(30):
                    P.act(small[:, 32:40], small[:, 40:48], AF.Identity, ['smallx'], ['smallx'], scale=1.0)
            if stage == 2:
                nq_ = 8 if not (3 <= sub < 4) else (int(round((sub - 3) * 10)) + 1 if sub != 3.9 else 2)
                if sub in (3.05, 3.06):
                    nq_ = 1
                for h in range(8 if sub >= 4 else 1):
                    P.load('sp', dbg[:, h * 512:h * 512 + nq_ * 64].bitcast(BF16), hmT[:, h, 0:nq_ * 128], [], 'dbg', reads=['hmT'])
            P.barrier()
        if stage == 2:
            P.finish()
            P.emit()
            holder['nc'] = nc
            return nc
        obT = sbuf(smix, "obT", [128, 8, 1024], BF16)


        with ExitStack() as s3:
            wqb = [sbuf(s3, "wqb%d" % i, [128, KC, 128], BF16) for i in range(2)]
            wkb = [sbuf(s3, "wkb%d" % i, [128, KC, 128], BF16) for i in range(2)]
            wvb = [sbuf(s3, "wvb%d" % i, [128, KC, 128], BF16) for i in range(2)]
            qbT = sbuf(s3, "qbT", [128, 1024], BF16)
            kbT = sbuf(s3, "kbT", [128, 2048], BF16)
            vb = sbuf(s3, "vb", [128, 16, 136], BF16)
            sqj = sbuf(s3, "sqj", [128, 512], F32)
            qn = sbuf(s3, "qn", [128, 4, 128], F32)
            ssq4 = sbuf(s3, "ssq4", [128, 4], F32)
            rs4 = sbuf(s3, "rs4", [128, 4], F32)
            ksum = sbuf(s3, "ksum", [128, 8], F32)
            kmr = sbuf(s3, "kmr", [128, 8], F32)
            kmh = sbuf(s3, "kmh", [128, 8], BF16)
            kml = sbuf(s3, "kml", [128, 8], BF16)
            gm = sbuf(s3, "gm", [128, 64], F32)
            top8 = sbuf(s3, "top8", [128, 8, 8], F32)
            sel = sbuf(s3, "sel", [128, 64], F32)
            acc = sbuf(s3, "acc", [128, 136], F32)
            rden = sbuf(s3, "rden", [128, 1], F32)
            P.memset('pool', vb[:, :, 128:136], 1.0, ['vb1'])

            def norm_T(b, dst, gcol, dkey):
                P.act(sqj[:], ps[b][:], AF.Square, PK(b), ['sqj'])
                P.reduce(ssq4[:], sqj[:].rearrange("p (t d) -> p t d", d=128), ALU.add, ['sqj'], ['ssq4'])
                rstd_from_ssq(ssq4[:], rs4[:], 4, 1.0 / 128, 'ssq4', 'rs4')
                for i in range(4):
                    P.ts('dve', qn[:, i, :], ps[b][:, i * 128:(i + 1) * 128], rs4[:, i:i + 1], None, ALU.mult, None,
                         PK(b) + ['rs4'], [('qn', i)])
                P.transposes([(ps[2][:, i * 128:(i + 1) * 128], qn[:, i, :]) for i in range(4)], ident,
                             [('qn', i) for i in range(4)] + ['cp'], PK(2))
                P.act(dst, ps[2][:], AF.Identity, PK(2) + ['cp', 'gqs'], [dkey], scale=gcol)

            gcnt = 0
            rcnt = 0
            for h in range(8):
                sl = h % 2
                P.load('pool', wqb[sl][:], win_v[:, :, O_QB + h * 128:O_QB + (h + 1) * 128], [('wqb', sl)], 'wqb%d' % sl)
                P.load('pool', wkb[sl][:], win_v[:, :, O_KB + h * 128:O_KB + (h + 1) * 128], [('wkb', sl)], 'wkb%d' % sl)
                P.load('pool', wvb[sl][:], win_v[:, :, O_VB + h * 128:O_VB + (h + 1) * 128], [('wvb', sl)], 'wvb%d' % sl)
                for tq in range(2):
                    b = 4 + tq % 2
                    P.mms([(ps[b][:, i * 128:(i + 1) * 128],
                            [(hT[:, k, 1024 + (tq * 4 + i) * 128:1024 + (tq * 4 + i + 1) * 128], wqb[sl][:, k, :])
                             for k in range(KC)]) for i in range(4)], hTk + [('wqb', sl)], PK(b))
                    norm_T(b, qbT[:, tq * 512:(tq + 1) * 512], gqs[:, 0:1], 'qbT')
                for tq in range(4):
                    b = 4 + tq % 2
                    P.mms([(ps[b][:, i * 128:(i + 1) * 128],
                            [(hT[:, k, (tq * 4 + i) * 128:(tq * 4 + i + 1) * 128], wkb[sl][:, k, :])
                             for k in range(KC)]) for i in range(4)], hTk + [('wkb', sl)], PK(b))
                    norm_T(b, kbT[:, tq * 512:(tq + 1) * 512], cp[:, C_GK:C_GK + 1], 'kbT')
                for tq in range(4):
                    b = 4 + tq % 2
                    P.mms([(ps[b][:, i * 128:(i + 1) * 128],
                            [(hT[:, k, (tq * 4 + i) * 128:(tq * 4 + i + 1) * 128], wvb[sl][:, k, :])
                             for k in range(KC)]) for i in range(4)], hTk + [('wvb', sl)], PK(b))
                    P.copy('dve', vb[:, tq * 4:(tq + 1) * 4, 0:128],
                           ps[b][:].rearrange("p (t d) -> p t d", d=128), PK(b), ['vb'])
                P.reduce(ksum[:], kbT[:].rearrange("p (n k) -> p n k", k=256), ALU.add, ['kbT'], ['ksum'])
                P.ts('dve', kmr[:], ksum[:], 1.0 / 256, None, ALU.mult, None, ['ksum'], ['kmr'])
                P.copy('dve', kmh[:], kmr[:], ['kmr'], ['kmh'])
                P.tt('dve', kml[:], kmr[:], kmh[:], ALU.subtract, ['kmr', 'kmh'], ['kml'])
                P.mms([(ps[3][:, Tq * 8:(Tq + 1) * 8],
                        [(qbT[:, Tq * 128:(Tq + 1) * 128], kmh[:]), (qbT[:, Tq * 128:(Tq + 1) * 128], kml[:])])
                       for Tq in range(8)], ['qbT', 'kmh', 'kml'], PK(3))
                P.tt('dve', gm[:], ps[3][:, 0:64], cp[:, C_GMASK:C_GMASK + 64], ALU.add, PK(3) + ['cp'], ['gm'])
                for Tq in range(8):
                    P.op('dve', lambda e, Tq=Tq: e.max(out=top8[:, Tq, :], in_=gm[:, Tq * 8:(Tq + 1) * 8]),
                         ['gm'], [('top8', Tq)])
                    P.ts('dve', sel[:, Tq * 8:(Tq + 1) * 8], gm[:, Tq * 8:(Tq + 1) * 8], top8[:, Tq, 2:3], None,
                         ALU.is_ge, None, ['gm', ('top8', Tq)], ['sel'])
                P.tt('dve', sel[:], sel[:], cp[:, C_VALID:C_VALID + 64], ALU.mult, ['sel', 'cp'], ['sel'])
                for Tq in range(8):
                    T = 8 + Tq
                    ntile = T + 1
                    for g0 in range(0, ntile, 4):
                        Ss = list(range(g0, min(g0 + 4, ntile)))
                        nj = len(Ss)
                        b = gcnt % 2
                        gcnt += 1
                        P.mms([(ps[b][:, j * 128:(j + 1) * 128],
                                [(kbT[:, S * 128:(S + 1) * 128], qbT[:, Tq * 128:(Tq + 1) * 128])])
                               for j, S in enumerate(Ss)], ['kbT', 'qbT'], PK(b))
                        P.act(PT[:, g0:g0 + nj, :], ps[b][:, 0:nj * 128].rearrange("p (t d) -> p t d", d=128),
                              AF.Exp, PK(b), [('PT', S) for S in Ss])
                    P.tt('dve', PT[:, T, :], PT[:, T, :], maskb[:], ALU.mult, [('PT', T), 'maskb'], [('PT', T)])
                    nown = T // 2
                    for n in range(nown + 1):
                        tiles = [S for S in (2 * n, 2 * n + 1) if S <= T]
                        r = rcnt % 4
                        rcnt += 1
                        reg = ps[6 + r % 2][:, (r // 2) * 136:(r // 2) * 136 + 136]
                        P.mm(reg, [(PT[:, S, :], vb[:, S, :]) for S in tiles],
                             [('PT', S) for S in tiles] + ['vb', 'vb1'], [('pvr', r)])
                        if n == nown:
                            P.tt('dve', acc[:], reg, acc[:], ALU.add, [('pvr', r), 'acc'], ['acc'])
                        elif n == 0:
                            P.ts('dve', acc[:], reg, sel[:, Tq * 8 + n:Tq * 8 + n + 1], None, ALU.mult, None,
                                 [('pvr', r), 'sel'], ['acc'])
                        else:
                            P.stt('dve', acc[:], reg, sel[:, Tq * 8 + n:Tq * 8 + n + 1], acc[:], ALU.mult, ALU.add,
                                  [('pvr', r), 'sel', 'acc'], ['acc'])
                    P.recip(rden[:], acc[:, 128:129], ['acc'], ['rden'])
                    P.ts('dve', hmt[:], acc[:, 0:128], rden[:, 0:1], None, ALU.mult, None, ['acc', 'rden'], ['hmt'])
                    P.mm(ps[2][:, 0:128], [(hmt[:], identb[:])], ['hmt', 'identb'], PK(2))
                    P.copy('act', obT[:, h, Tq * 128:(Tq + 1) * 128], ps[2][:, 0:128], PK(2), ['obT'])
            if stage == 3:
                for h in range(8):
                    P.load('sp', dbg[:, h * 512:(h + 1) * 512].bitcast(BF16), obT[:, h, :], [], 'dbg', reads=['obT'])
            P.barrier()
        if stage == 3:
            P.finish()
            P.emit()
            holder['nc'] = nc
            return nc


        yT = sbuf(smix, "yT", [128, KC, 1024], BF16)
        wa_v = d_wa.rearrange("(k p) n -> p k n", p=128)
        wb_v = d_wb.rearrange("(k p) n -> p k n", p=128)
        with ExitStack() as s4:
            wga = sbuf(s4, "wga", [128, KC, 256], BF16)
            wgb = sbuf(s4, "wgb", [128, KC, 256], BF16)
            wa4 = sbuf(s4, "wa4", [128, 8, 256], BF16)
            wb4 = sbuf(s4, "wb4", [128, 8, 256], BF16)
            sga = sbuf(s4, "sga", [128, 512], F32)
            sgb = sbuf(s4, "sgb", [128, 512], F32)
            t1 = sbuf(s4, "t1", [128, 512], F32)
            cnt = 0
            for cg in range(8):
                P.load('pool', wga[:], win_v[:, :, O_GA + cg * 256:O_GA + (cg + 1) * 256], ['wga'], 'wga')
                P.load('pool', wgb[:], win_v[:, :, O_GB + cg * 256:O_GB + (cg + 1) * 256], ['wgb'], 'wgb')
                P.load('pool', wa4[:], wa_v[:, :, cg * 256:(cg + 1) * 256], ['wa4'], 'wa4')
                P.load('pool', wb4[:], wb_v[:, :, cg * 256:(cg + 1) * 256], ['wb4'], 'wb4')
                for jj in range(2):
                    j = cg * 2 + jj
                    cs = slice(jj * 128, (jj + 1) * 128)
                    for g in range(2):
                        bga, bgb, bya, byb = (0, 1, 4, 5) if cnt % 2 == 0 else (2, 3, 6, 7)
                        cnt += 1
                        ts_ = slice(g * 512, (g + 1) * 512)
                        to_ = slice(1024 + g * 512, 1024 + (g + 1) * 512)
                        P.mm(ps[bga][:], [(wga[:, k, cs], hT[:, k, to_]) for k in range(KC)], ['wga'] + hTk, PK(bga))
                        P.mm(ps[bgb][:], [(wgb[:, k, cs], hT[:, k, to_]) for k in range(KC)], ['wgb'] + hTk, PK(bgb))
                        P.mm(ps[bya][:], [(wa4[:, k, cs], hmT[:, k, ts_]) for k in range(8)], ['wa4', 'hmT'], PK(bya))
                        P.mm(ps[byb][:], [(wb4[:, k, cs], obT[:, k, ts_]) for k in range(8)], ['wb4', 'obT'], PK(byb))
                        P.act(sga[:], ps[bga][:], AF.Sigmoid, PK(bga), ['sga'])
                        P.act(sgb[:], ps[bgb][:], AF.Sigmoid, PK(bgb), ['sgb'])
                        P.tt('dve', t1[:], sga[:], ps[bya][:], ALU.mult, ['sga'] + PK(bya), ['t1'])
                        P.tt('dve', sgb[:], sgb[:], ps[byb][:], ALU.mult, ['sgb'] + PK(byb), ['sgb'])
                        P.tt('dve', yT[:, j, ts_], t1[:], sgb[:], ALU.add, ['t1', 'sgb'], ['yT'])
            P.barrier()

        wout_v = d_wout.rearrange("(k p) n -> p k n", p=128)

        def x1k(tile, c0, c1):
            return [('x1', tile, c) for c in range(c0, c1)]
        with ExitStack() as s5:
            wo4 = [sbuf(s5, "wo4%d" % i, [128, KC, 512], BF16) for i in range(2)]
            xr = [sbuf(s5, "xr%d" % i, [128, 512], F32) for i in range(2)]
            tmpo = sbuf(s5, "tmpo", [128, 512], F32)
            for cg in range(4):
                sl = cg % 2
                P.load('pool', wo4[sl][:], wout_v[:, :, cg * 512:(cg + 1) * 512], [('wo4', sl)], 'wo4%d' % sl)
                for tile in range(8):
                    b = tile % 2
                    xs_ = tile % 2
                    P.load('sp', xr[xs_][:], d_xown[tile * 128:(tile + 1) * 128, cg * 512:(cg + 1) * 512],
                           [('xr', xs_)], 'xr%d' % xs_)
                    P.mm(ps[b][:], [(yT[:, k, tile * 128:(tile + 1) * 128], wo4[sl][:, k, :]) for k in range(KC)],
                         ['yT', ('wo4', sl)], PK(b))
                    P.tt('dve', tmpo[:], ps[b][:], gt1[:, cg * 512:(cg + 1) * 512], ALU.mult, PK(b) + gt1k, ['tmpo'])
                    P.tt('dve', x1[:, tile, cg * 512:(cg + 1) * 512], tmpo[:], xr[xs_][:], ALU.add,
                         ['tmpo', ('xr', xs_)], x1k(tile, cg * 2, cg * 2 + 2))
            if stage == 4:
                for tile in range(8):
                    P.load('sp', dbg[:, tile * 2048:(tile + 1) * 2048], x1[:, tile, :], [], 'dbg',
                           reads=x1k(tile, 0, 8))
            P.barrier()
        if stage == 4:
            P.finish()
            P.emit()
            holder['nc'] = nc
            return nc
        try:
            smix.close()
        except AssertionError:
            pass
        P.barrier()

        with ExitStack() as s6:
            tT = sbuf(s6, "tT", [128, KC, 1024], BF16)
            actT = sbuf(s6, "actT", [128, KC, 1024], BF16)
            with ExitStack() as s6a:
                xs = sbuf(s6a, "xs", [128, D], F32)
                wr = sbuf(s6a, "wr", [128, KC, NEXP], BF16)
                lg = sbuf(s6a, "lg", [128, 8, NEXP], F32)
                ex = sbuf(s6a, "ex", [128, NEXP], F32)
                mk = sbuf(s6a, "mk", [128, NEXP], F32)
                t8 = sbuf(s6a, "t8", [128, 8, 8], F32)
                ssqm = sbuf(s6a, "ssqm", [128, 8], F32)
                rstm = sbuf(s6a, "rstm", [128, 8], F32)
                sm2 = sbuf(s6a, "sm2", [128, 8], F32)
                combT = sbuf(s6a, "combT", [32, 8, 128], F32)
                bdn = sbuf(s6a, "bdn", [32, D], F32)
                tmpb = sbuf(s6a, "tmpb", [128, 512], F32)
                P.load('pool', wr[:], d_wr.rearrange("(k p) n -> p k n", p=128), ['wr'], 'wr')
                P.load('sp', bdn[:], d_bdn, ['bdn'], 'bdn')
                for tile in range(8):
                    P.act(xs[:], x1[:, tile, :], AF.Square, x1k(tile, 0, 8), ['xs', ('ssqm', tile)],
                          accum_out=ssqm[:, tile:tile + 1])
                    rstd_from_ssq(ssqm[:, tile:tile + 1], rstm[:, tile:tile + 1], 1, 1.0 / D, ('ssqm', tile), ('rstm', tile))
                    P.ts('dve', xs[:], x1[:, tile, :], rstm[:, tile:tile + 1], None, ALU.mult, None,
                         x1k(tile, 0, 8) + [('rstm', tile)], ['xs'])
                    for kg in range(4):
                        b = kg % 2
                        P.transposes([(ps[b][:, i * 128:(i + 1) * 128], xs[:, (kg * 4 + i) * 128:(kg * 4 + i + 1) * 128])
                                      for i in range(4)], ident, ['xs', 'cp'], PK(b))
                        for i in range(4):
                            k = kg * 4 + i
                            P.act(tT[:, k, tile * 128:(tile + 1) * 128], ps[b][:, i * 128:(i + 1) * 128], AF.Identity,
                                  [('ps', b, i), 's2c', 'modc'], ['tT'], scale=s2c[:, k:k + 1], bias=modc[:, 48 + k:49 + k])
                P.mms([(ps[2][:, tile * 32:(tile + 1) * 32],
                        [(tT[:, k, tile * 128:(tile + 1) * 128], wr[:, k, :]) for k in range(KC)]) for tile in range(8)],
                      ['tT', 'wr'], PK(2))
                P.tt('dve', lg[:], ps[2][:, 0:256].rearrange("p (t e) -> p t e", e=NEXP),
                     cp[:, C_BROUT:C_BROUT + NEXP].unsqueeze(1).to_broadcast([128, 8, NEXP]), ALU.add,
                     PK(2) + ['cp'], ['lg'])
                for tile in range(8):
                    P.op('dve', lambda e, tile=tile: e.max(out=t8[:, tile, :], in_=lg[:, tile, :]), ['lg'], [('t8', tile)])
                    P.ts('dve', sm2[:, 0:1], t8[:, tile, 0:1], -1.0, None, ALU.mult, None, [('t8', tile)], ['sm2'])
                    P.act(ex[:], lg[:, tile, :], AF.Exp, ['lg', 'sm2'], ['ex'], bias=sm2[:, 0:1], scale=1.0)
                    P.ts('dve', mk[:], lg[:, tile, :], t8[:, tile, 3:4], None, ALU.is_ge, None, ['lg', ('t8', tile)], ['mk'])
                    P.tt('dve', ex[:], ex[:], mk[:], ALU.mult, ['ex', 'mk'], ['ex'])
                    P.reduce(sm2[:, 1:2], ex[:], ALU.add, ['ex'], ['sm2'])
                    P.recip(sm2[:, 2:3], sm2[:, 1:2], ['sm2'], ['sm2'])
                    P.ts('dve', comb[:, tile, :], ex[:], sm2[:, 2:3], None, ALU.mult, None, ['ex', 'sm2'], [('comb', tile)])
                combk = [('comb', t_) for t_ in range(8)]
                for half in range(2):
                    P.transposes([(ps[3 + half][0:32, i * 128:(i + 1) * 128], comb[:, half * 4 + i, :]) for i in range(4)],
                                 ident, combk + ['cp'], PK(3 + half))
                    P.copy('dve', combT[:, half * 4:(half + 1) * 4, :],
                           ps[3 + half][0:32, :].rearrange("p (t d) -> p t d", d=128), PK(3 + half), ['combT'])
                for tile in range(8):
                    for cg in range(4):
                        b = 5 + (tile * 4 + cg) % 2
                        P.mm(ps[b][:], [(combT[:, tile, :], bdn[:, cg * 512:(cg + 1) * 512])], ['combT', 'bdn'], PK(b))
                        P.tt('dve', tmpb[:], ps[b][:], gt2[:, cg * 512:(cg + 1) * 512], ALU.mult, PK(b) + gt2k, ['tmpb'])
                        P.tt('dve', x1[:, tile, cg * 512:(cg + 1) * 512], tmpb[:], x1[:, tile, cg * 512:(cg + 1) * 512],
                             ALU.add, ['tmpb'] + x1k(tile, cg * 2, cg * 2 + 2), x1k(tile, cg * 2, cg * 2 + 2))
                P.barrier()
            with ExitStack() as s6b:
                wupg = [sbuf(s6b, "wupg%d" % i, [128, KC, 256], BF16) for i in range(2)]
                wupl = [sbuf(s6b, "wupl%d" % i, [128, KC, 256], BF16) for i in range(2)]
                wdn = sbuf(s6b, "wdn", [128, KC, 256], BF16)
                bup = sbuf(s6b, "bup", [128, NEXP * 32], F32)
                g_ = sbuf(s6b, "g_", [128, 512], F32)
                sg = sbuf(s6b, "sg", [128, 512], F32)
                l_ = sbuf(s6b, "l_", [128, 512], F32)
                tmpd = sbuf(s6b, "tmpd", [128, 256], F32)
                P.load('sp', bup[:], d_bup, ['bup'], 'bup')
                cnt = 0
                scnt = 0
                for e_ in range(NEXP):
                    wup_e = d_wup[e_].rearrange("(k p) n -> p k n", p=128)
                    wdn_e = d_wdn[e_].rearrange("(k p) n -> p k n", p=128)
                    for mp2 in range(8):
                        st = scnt % 2
                        scnt += 1
                        P.load('pool', wupg[st][:], wup_e[:, :, mp2 * 256:(mp2 + 1) * 256], [('wupg', st)], 'wupg%d' % st)
                        P.load('pool', wupl[st][:], wup_e[:, :, D + mp2 * 256:D + (mp2 + 1) * 256], [('wupl', st)],
                               'wupl%d' % st)
                        for mi in range(2):
                            mp = mp2 * 2 + mi
                            cs = slice(mi * 128, (mi + 1) * 128)
                            for g in range(2):
                                bA, bB = (0, 1) if cnt % 2 == 0 else (2, 3)
                                cnt += 1
                                ts_ = slice(g * 512, (g + 1) * 512)
                                P.mm(ps[bA][:], [(wupg[st][:, k, cs], tT[:, k, ts_]) for k in range(KC)],
                                     [('wupg', st), 'tT'], PK(bA))
                                P.mm(ps[bB][:], [(wupl[st][:, k, cs], tT[:, k, ts_]) for k in range(KC)],
                                     [('wupl', st), 'tT'], PK(bB))
                                P.ts('dve', g_[:], ps[bA][:], bup[:, e_ * 32 + mp:e_ * 32 + mp + 1], 7.0, ALU.add, ALU.min,
                                     PK(bA) + ['bup'], ['g_'])
                                P.act(sg[:], g_[:], AF.Sigmoid, ['g_'], ['sg'], scale=1.702)
                                P.ts('dve', l_[:], ps[bB][:], bup[:, e_ * 32 + 16 + mp:e_ * 32 + 17 + mp], -7.0,
                                     ALU.add, ALU.max, PK(bB) + ['bup'], ['l_'])
                                P.ts('dve', l_[:], l_[:], 7.0, 1.0, ALU.min, ALU.add, ['l_'], ['l_'])
                                P.tt('dve', g_[:], g_[:], sg[:], ALU.mult, ['g_', 'sg'], ['g_'])
                                P.tt('dve', actT[:, mp, ts_], g_[:], l_[:], ALU.mult, ['g_', 'l_'], ['actT'])
                    for cgd in range(8):
                        P.load('pool', wdn[:], wdn_e[:, :, cgd * 256:(cgd + 1) * 256], ['wdn'], 'wdn')
                        for tile in range(8):
                            b = 4 + tile % 2
                            P.mm(ps[b][:, 0:256], [(actT[:, k, tile * 128:(tile + 1) * 128], wdn[:, k, :]) for k in range(KC)],
                                 ['actT', 'wdn'], PK(b))
                            P.tt('dve', tmpd[:], ps[b][:, 0:256], gt2[:, cgd * 256:(cgd + 1) * 256], ALU.mult,
                                 PK(b) + gt2k, ['tmpd'])
                            P.stt('dve', x1[:, tile, cgd * 256:(cgd + 1) * 256], tmpd[:], comb[:, tile, e_:e_ + 1],
                                  x1[:, tile, cgd * 256:(cgd + 1) * 256], ALU.mult, ALU.add,
                                  ['tmpd', ('comb', tile)] + x1k(tile, cgd, cgd + 1), x1k(tile, cgd, cgd + 1))
                for tile in range(8):
                    P.load('sp', d_out[tile * 128:(tile + 1) * 128, :], x1[:, tile, :], [], 'out%d' % (tile % 2),
                           reads=x1k(tile, 0, 8))
        P.finish()
        P.emit()
        holder['nc'] = nc
        return nc


def _consts():
    cp = np.zeros((128, C_TOT), np.float32)
    cp[:, C_ID:C_ID + 128] = np.eye(128, dtype=np.float32)
    cp[:, C_TRI:C_TRI + 128] = np.triu(np.ones((128, 128), np.float32))
    cp[:, C_ONES:C_ONES + 128] = 1.0
    cp[127, C_S127:C_S127 + 128] = 1.0
    cp[:, C_MASK:C_MASK + 128] = np.triu(np.ones((128, 128), np.float32))
    cp[:64, C_HM0] = 0.125
    cp[64:, C_HM1] = 0.125
    return cp


def _prep_inputs(inputs):
    x = np.asarray(inputs["x"], np.float32)
    c = np.asarray(inputs["c"], np.float32)
    L = 0
    cp0 = _consts()
    shared = {
        "gml_bc": np.ascontiguousarray(np.broadcast_to(
            np.asarray(inputs["g_mlstm_out"], np.float32)[L].reshape(1, 1024), (128, 1024))),
        "w_ada": np.ascontiguousarray(inputs["w_ada"][L], np.float32),
        "b_ada": np.ascontiguousarray(inputs["b_ada"][L].reshape(1, -1), np.float32),
        "w_in": np.ascontiguousarray(inputs["w_in"][L], np.float32),
        "w_branch_a": np.ascontiguousarray(inputs["w_branch_a"][L], np.float32),
        "w_branch_b": np.ascontiguousarray(inputs["w_branch_b"][L], np.float32),
        "w_out": np.ascontiguousarray(inputs["w_out"][L], np.float32),
        "w_router": np.ascontiguousarray(inputs["w_router"][L], np.float32),
        "w_up": np.ascontiguousarray(inputs["w_up"][L], np.float32),
        "b_up_col": np.ascontiguousarray(
            np.asarray(inputs["b_up"], np.float32)[L].reshape(NEXP, 32, 128).transpose(2, 0, 1).reshape(128, NEXP * 32)),
        "w_down": np.ascontiguousarray(inputs["w_down"][L], np.float32),
        "b_down": np.ascontiguousarray(inputs["b_down"][L], np.float32),
    }
    col = lambda v: np.asarray(v, np.float32).reshape(16, 128).T
    in_maps = []
    for core in range(8):
        b, half = core // 2, core % 2
        cp = cp0.copy()
        cp[:, C_CCOL:C_CCOL + 16] = col(c[b])
        cp[:, C_GMIX:C_GMIX + 16] = col(inputs["g_mix"][L])
        cp[:, C_GFFN:C_GFFN + 16] = col(inputs["g_ffn"][L])
        cp[:, C_BIF:C_BIF + 8] = np.asarray(inputs["b_igate"], np.float32)[L][None, :]
        cp[:, C_BIF + 8:C_BIF + 16] = np.asarray(inputs["b_fgate"], np.float32)[L][None, :]
        cp[:, C_GQ] = np.asarray(inputs["g_q"], np.float32)[L]
        cp[:, C_GK] = np.asarray(inputs["g_k"], np.float32)[L]
        cp[:, C_FLAG] = float(half)
        valid = np.zeros((8, 8), np.float32)
        for tq in range(8):
            for n in range(8):
                if n < 4:
                    valid[tq, n] = float(half)
                else:
                    valid[tq, n] = 1.0 if (n - 4) < tq // 2 else 0.0
        cp[:, C_VALID:C_VALID + 64] = valid.reshape(1, 64)
        cp[:, C_GMASK:C_GMASK + 64] = np.where(valid.reshape(1, 64) > 0, 0.0, -1e30)
        cp[:, C_BROUT:C_BROUT + 32] = np.asarray(inputs["b_router"], np.float32)[L][None, :]
        m = dict(shared)
        m["cpack"] = cp
        m["x_own"] = np.ascontiguousarray(x[b, half * 1024:(half + 1) * 1024])
        m["x_ctx"] = np.ascontiguousarray(x[b, 0:1024]) if half == 1 else np.zeros((1024, D), np.float32)
        in_maps.append(m)
    return in_maps


def kernel(**inputs):
    in_maps = _prep_inputs(inputs)
    nc = build_program()
    res = run_bass_kernel_spmd(nc, in_maps, core_ids=list(range(8)))
    out = np.zeros((4, 2048, D), np.float32)
    for core in range(8):
        b, half = core // 2, core % 2
        out[b, half * 1024:(half + 1) * 1024] = res.results[core]["out"]
    return out
```

```python
import numpy as np
from contextlib import ExitStack
import concourse.bass as bass
import concourse.mybir as mybir
from concourse.bass_utils import run_bass_kernel_spmd

F32 = mybir.dt.float32
BF16 = mybir.dt.bfloat16
AF = mybir.ActivationFunctionType
ALU = mybir.AluOpType
AX = mybir.AxisListType

D = 2048
KC = 16
NEXP = 32
EPS = 1e-6

O_QA, O_KA, O_VA, O_OA, O_IA, O_FA = 0, 512, 1024, 2048, 3072, 3080
O_QB, O_KB, O_VB, O_GA, O_GB = 3088, 4112, 5136, 6160, 8208
D_IN = 10256

C_ID, C_TRI, C_ONES, C_S127, C_MASK = 0, 128, 256, 384, 512
C_CCOL, C_GMIX, C_GFFN, C_BIF, C_GQ, C_GK, C_FLAG = 640, 656, 672, 688, 704, 705, 706
C_GMASK, C_VALID, C_BROUT = 708, 772, 836
C_HM0, C_HM1 = 868, 869
C_TOT = 870


class Prog:
    ENGS = ('pe', 'act', 'dve', 'pool', 'sp')

    def __init__(self, nc, es):
        self.nc = nc
        self.es = es
        self.q = {e: [] for e in self.ENGS}
        self.cnt = {}
        self.sems = {}
        self.last_w = {}
        self.readers = {}
        self.seen = {e: {} for e in self.ENGS}
        for e in self.ENGS:
            self._sem(e)

    def _sem(self, name):
        if name not in self.sems:
            self.sems[name] = self.es.enter_context(self.nc.semaphore(name))
            self.cnt[name] = 0
        return self.sems[name]

    def _deps(self, eng, reads, writes):
        deps = {}

        def add(ev):
            if ev is None:
                return
            s, v = ev
            if deps.get(s, 0) < v:
                deps[s] = v
        for k in reads:
            add(self.last_w.get(k))
        for k in writes:
            add(self.last_w.get(k))
            for s, v in self.readers.get(k, {}).items():
                add((s, v))
        waits = []
        for s, v in deps.items():
            if s == 'pe' and eng == 'pe':
                continue
            if self.seen[eng].get(s, 0) >= v:
                continue
            self.seen[eng][s] = v
            waits.append((s, v))
        return waits

    def _record(self, ev, reads, writes):
        for k in reads:
            d = self.readers.setdefault(k, {})
            if d.get(ev[0], 0) < ev[1]:
                d[ev[0]] = ev[1]
        for k in writes:
            self.last_w[k] = ev
            self.readers[k] = {}

    def op(self, eng, fn, reads=(), writes=()):
        waits = self._deps(eng, reads, writes)
        self.cnt[eng] += 1
        ev = (eng, self.cnt[eng])
        self._record(ev, reads, writes)
        self.q[eng].append((waits, fn, (eng, 1)))

    def dma(self, queue, fn, reads, writes, slot):
        sem = 'd_' + slot
        self._sem(sem)
        waits = self._deps(queue, reads, writes)
        prev = self.cnt[sem]
        if prev > 0 and self.seen[queue].get(sem, 0) < prev:
            self.seen[queue][sem] = prev
            waits.append((sem, prev))
        self.cnt[sem] += 16
        ev = (sem, self.cnt[sem])
        self._record(ev, reads, writes)
        self.q[queue].append((waits, fn, (sem, 16)))

    def barrier(self):
        allv = dict(self.cnt)
        for e in self.ENGS:
            waits = []
            for s, v in allv.items():
                if v == 0 or s == e:
                    continue
                if self.seen[e].get(s, 0) >= v:
                    continue
                self.seen[e][s] = v
                waits.append((s, v))
            if waits:
                self.q[e].append((waits, None, None))

    def finish(self):
        waits = [(s, v) for s, v in self.cnt.items() if v > 0 and s != 'sp']
        self.q['sp'].append((waits, None, None))

    def emit(self):
        nc = self.nc
        sems = self.sems

        def replay(name, eng):
            for waits, fn, inc in self.q[name]:
                for s, v in waits:
                    eng.wait_ge(sems[s], v)
                if fn is None:
                    continue
                ins = fn(eng)
                ins.then_inc(sems[inc[0]], inc[1])
        with nc.Block() as block:
            @block.tensor
            def _(eng):
                replay('pe', eng)

            @block.scalar
            def _(eng):
                replay('act', eng)

            @block.vector
            def _(eng):
                replay('dve', eng)

            @block.gpsimd
            def _(eng):
                replay('pool', eng)

            @block.sync
            def _(eng):
                replay('sp', eng)

    def mm(self, out, pairs, reads, writes):
        pairs = list(pairs)

        def fn(e):
            n = len(pairs)
            ins = None
            for i, (l, r) in enumerate(pairs):
                ins = e.matmul(out, lhsT=l, rhs=r, start=(i == 0), stop=(i == n - 1))
            return ins
        self.op('pe', fn, reads, writes)

    def mms(self, groups, reads, writes):
        groups = [(o, list(p)) for o, p in groups]

        def fn(e):
            ins = None
            for o, pairs in groups:
                n = len(pairs)
                for i, (l, r) in enumerate(pairs):
                    ins = e.matmul(o, lhsT=l, rhs=r, start=(i == 0), stop=(i == n - 1))
            return ins
        self.op('pe', fn, reads, writes)

    def transposes(self, items, ident, reads, writes):
        items = list(items)

        def fn(e):
            ins = None
            for o, i in items:
                ins = e.transpose(out=o, in_=i, identity=ident)
            return ins
        self.op('pe', fn, reads, writes)

    def act(self, out, in_, func, reads, writes, **kw):
        self.op('act', lambda e: e.activation(out=out, in_=in_, func=func, **kw), reads, writes)

    def ts(self, eng, out, in0, s1, s2, op0, op1, reads, writes, **kw):
        if op1 is None:
            self.op(eng, lambda e: e.tensor_scalar(out=out, in0=in0, scalar1=s1, scalar2=None, op0=op0, **kw),
                    reads, writes)
        else:
            self.op(eng, lambda e: e.tensor_scalar(out=out, in0=in0, scalar1=s1, scalar2=s2, op0=op0, op1=op1, **kw),
                    reads, writes)

    def tt(self, eng, out, in0, in1, op, reads, writes):
        self.op(eng, lambda e: e.tensor_tensor(out=out, in0=in0, in1=in1, op=op), reads, writes)

    def stt(self, eng, out, in0, scalar, in1, op0, op1, reads, writes):
        self.op(eng, lambda e: e.scalar_tensor_tensor(out=out, in0=in0, scalar=scalar, in1=in1, op0=op0, op1=op1),
                reads, writes)

    def copy(self, eng, out, in_, reads, writes):
        if eng == 'act':
            self.op('act', lambda e: e.copy(out=out, in_=in_), reads, writes)
        else:
            self.op(eng, lambda e: e.tensor_copy(out=out, in_=in_), reads, writes)

    def recip(self, out, in_, reads, writes):
        self.op('dve', lambda e: e.reciprocal(out=out, in_=in_), reads, writes)

    def reduce(self, out, in_, op, reads, writes):
        self.op('dve', lambda e: e.tensor_reduce(out=out, in_=in_, axis=AX.X, op=op), reads, writes)

    def memset(self, eng, ap, val, writes):
        self.op(eng, lambda e: e.memset(ap, val), [], writes)

    def load(self, queue, out, in_, writes, slot, reads=()):
        self.dma(queue, lambda e: e.dma_start(out=out, in_=in_), list(reads), writes, slot)


def build_program(stage=99, sub=99):
    holder = {}
    try:
        holder['nc'] = _build_program(holder, stage, sub)
    except AssertionError as e:
        if 'non-stack order' not in str(e) or 'nc' not in holder:
            raise
    return holder['nc']


def _build_program(holder, stage=99, sub=99):
    nc = bass.Bass("TRN2", target_bir_lowering=False)

    def dr(name, shape, dt=F32, kind="ExternalInput"):
        return nc.dram_tensor(name, shape, dt, kind=kind).ap()

    d_xown = dr("x_own", [1024, D])
    d_xctx = dr("x_ctx", [1024, D])
    d_cpack = dr("cpack", [128, C_TOT])
    d_gml = dr("gml_bc", [128, 1024])
    d_wada = dr("w_ada", [D, 6 * D])
    d_bada = dr("b_ada", [1, 6 * D])
    d_win = dr("w_in", [D, D_IN])
    d_wa = dr("w_branch_a", [1024, D])
    d_wb = dr("w_branch_b", [1024, D])
    d_wout = dr("w_out", [D, D])
    d_wr = dr("w_router", [D, NEXP])
    if stage >= 5:
        d_wup = dr("w_up", [NEXP, D, 2 * D])
        d_bup = dr("b_up_col", [128, NEXP * 32])
        d_wdn = dr("w_down", [NEXP, D, D])
        d_bdn = dr("b_down", [NEXP, D])
    d_out = dr("out", [1024, D], kind="ExternalOutput")
    dbg = None
    if stage < 99:
        dbg = dr("dbg", [128, 16384], kind="ExternalOutput")

    win_v = d_win.rearrange("(k p) n -> p k n", p=128)

    with ExitStack() as es:
        P = Prog(nc, es)

        def sbuf(scope, name, shape, dt):
            return scope.enter_context(nc.sbuf_tensor(name, shape, dt))

        ps = [es.enter_context(nc.psum_tensor("ps%d" % i, [128, 512], F32)) for i in range(8)]

        def PK(b):
            return [('ps', b, j) for j in range(4)]

        cp = sbuf(es, "cp", [128, C_TOT], F32)
        identb = sbuf(es, "identb", [128, 128], BF16)
        maskb = sbuf(es, "maskb", [128, 128], BF16)
        modc = sbuf(es, "modc", [128, 96], F32)
        s1c = sbuf(es, "s1c", [128, 16], F32)
        s2c = sbuf(es, "s2c", [128, 16], F32)
        gt2 = sbuf(es, "gt2", [128, D], F32)
        gqs = sbuf(es, "gqs", [128, 1], F32)
        small = sbuf(es, "small", [128, 64], F32)
        comb = sbuf(es, "comb", [128, 8, NEXP], F32)
        big64 = sbuf(es, "big64", [128, 32768], BF16)
        smix = ExitStack()
        es.callback(smix.close)
        gt1 = sbuf(smix, "gt1", [128, D], F32)

        ident = cp[:, C_ID:C_ID + 128]
        tri = cp[:, C_TRI:C_TRI + 128]
        ones = cp[:, C_ONES:C_ONES + 128]
        s127 = cp[:, C_S127:C_S127 + 128]
        mask01 = cp[:, C_MASK:C_MASK + 128]
        flag = cp[:, C_FLAG:C_FLAG + 1]

        P.load('sp', cp[:], d_cpack, ['cp'], 'cp')
        P.copy('dve', identb[:], ident, ['cp'], ['identb'])
        P.copy('dve', maskb[:], mask01, ['cp'], ['maskb'])
        P.ts('dve', gqs[:], cp[:, C_GQ:C_GQ + 1], float(128 ** -0.5), None, ALU.mult, None, ['cp'], ['gqs'])

        def rstd_from_ssq(ssq_ap, out_ap, n, inv_n, key_in, key_out):
            tmp = small[:, 0:n]
            P.ts('dve', tmp, ssq_ap, float(inv_n), float(EPS), ALU.mult, ALU.add, [key_in], ['small'])
            P.act(tmp, tmp, AF.Sqrt, ['small'], ['small'])
            P.recip(out_ap, tmp, ['small'], [key_out])

        with ExitStack() as s0:
            wada = [sbuf(s0, "wada%d" % i, [128, KC, 256], F32) for i in range(2)]
            brow = [sbuf(s0, "brow%d" % i, [1, 256], F32) for i in range(2)]
            mrow = sbuf(s0, "mrow", [1, 6 * D], F32)
            scol = sbuf(s0, "scol", [128, 16], F32)
            P.act(scol[:], cp[:, C_CCOL:C_CCOL + 16], AF.Silu, ['cp'], ['scol'])
            wada_v = d_wada.rearrange("(k p) n -> p k n", p=128)
            for n in range(48):
                sl = n % 2
                P.load('sp', wada[sl][:], wada_v[:, :, n * 256:(n + 1) * 256], [('wada', sl)], 'wada%d' % sl)
                P.load('sp', brow[sl][:], d_bada[:, n * 256:(n + 1) * 256], [('brow', sl)], 'brow%d' % sl)
                b = n % 2
                P.mm(ps[b][0:1, 0:256], [(scol[:, k:k + 1], wada[sl][:, k, :]) for k in range(KC)],
                     ['scol', ('wada', sl)], PK(b))
                P.tt('dve', mrow[0:1, n * 256:(n + 1) * 256], ps[b][0:1, 0:256], brow[sl][0:1, :],
                     ALU.add, PK(b) + [('brow', sl)], [('mrow', n)])
            mrow_keys = [('mrow', n) for n in range(48)]
            P.mms([(ps[2][:, j:j + 1], [(mrow[0:1, j * 128:(j + 1) * 128], ones[0:1, 0:1])]) for j in range(96)],
                  mrow_keys + ['cp'], PK(2))
            P.copy('dve', modc[:], ps[2][:, 0:96], PK(2), ['modc'])
            for i in range(4):
                for (gt, off, nm) in ((gt1, 2 * D, 'gt1'), (gt2, 5 * D, 'gt2')):
                    b = 3 + (i % 2) * 2 + (0 if nm == 'gt1' else 1)
                    P.mm(ps[b][:], [(ones[0:1, 0:128], mrow[0:1, off + i * 512: off + (i + 1) * 512])],
                         mrow_keys + ['cp'], PK(b))
                    P.copy('act', gt[:, i * 512:(i + 1) * 512], ps[b][:], PK(b), [(nm, i)])
            P.stt('dve', s1c[:], modc[:, 16:32], 1.0, cp[:, C_GMIX:C_GMIX + 16], ALU.add, ALU.mult,
                  ['modc', 'cp'], ['s1c'])
            P.stt('dve', s2c[:], modc[:, 64:80], 1.0, cp[:, C_GFFN:C_GFFN + 16], ALU.add, ALU.mult,
                  ['modc', 'cp'], ['s2c'])
            if stage == 0 and sub != 99:
                for i_ in range(int(sub)):
                    P.mm(ps[7][:, 0:128], [(identb[:], identb[:])], ['identb'], PK(7))
            if stage == 0:
                P.load('sp', dbg[:, 0:96], modc[:], [], 'dbg', reads=['modc'])
                P.load('sp', dbg[:, 96:112], s1c[:], [], 'dbg', reads=['s1c'])
                P.load('sp', dbg[:, 2048:4096], gt1[:], [], 'dbg', reads=[('gt1', i) for i in range(4)])
                P.load('sp', dbg[:, 4096:6144], gt2[:], [], 'dbg', reads=[('gt2', i) for i in range(4)])
            P.barrier()
        gt1k = [('gt1', i) for i in range(4)]
        gt2k = [('gt2', i) for i in range(4)]
        if stage == 0:
            P.finish()
            P.emit()
            holder['nc'] = nc
            return nc

        hT = big64[:].rearrange("p (k t) -> p k t", k=KC)
        x1 = big64[:].bitcast(F32).rearrange("p (t d) -> p t d", t=8)
        hmT = sbuf(smix, "hmT", [128, 8, 1024], BF16)
        ssq = sbuf(smix, "ssq", [128, 16], F32)
        rstd = sbuf(smix, "rstd", [128, 16], F32)

        with ExitStack() as s1:
            xt = [sbuf(s1, "xt%d" % i, [128, D], F32) for i in range(8)]
            junk = sbuf(s1, "junk", [128, D], F32)
            for tg in range(4):
                for i in range(4):
                    ti = tg * 4 + i
                    sl = ti % 8
                    src = d_xctx if ti < 8 else d_xown
                    r0 = (ti % 8) * 128
                    P.load('sp', xt[sl][:], src[r0:r0 + 128, :], [('xt', sl)], 'xt%d' % sl)
                    P.act(junk[:], xt[sl][:], AF.Square, [('xt', sl)], ['junk', ('ssq', ti)],
                          accum_out=ssq[:, ti:ti + 1])
                    rstd_from_ssq(ssq[:, ti:ti + 1], rstd[:, ti:ti + 1], 1, 1.0 / D, ('ssq', ti), ('rstd', ti))
                    P.ts('dve', xt[sl][:], xt[sl][:], rstd[:, ti:ti + 1], None, ALU.mult, None,
                         [('xt', sl), ('rstd', ti)], [('xt', sl)])
                for k in range(KC):
                    b = k % 2
                    P.transposes([(ps[b][:, i * 128:(i + 1) * 128], xt[(tg * 4 + i) % 8][:, k * 128:(k + 1) * 128])
                                  for i in range(4)], ident,
                                 [('xt', (tg * 4 + i) % 8) for i in range(4)] + ['cp'], PK(b))
                    P.act(hT[:, k, tg * 512:(tg + 1) * 512], ps[b][:], AF.Identity, PK(b) + ['s1c', 'modc'],
                          [('hT', tg)], scale=s1c[:, k:k + 1], bias=modc[:, k:k + 1])
            if stage == 1 and sub != 99:
                for i_ in range(int(sub)):
                    P.mm(ps[3][:, 0:128], [(identb[:], identb[:])], ['identb'], PK(3))
            if stage == 1:
                for k in range(KC):
                    P.load('sp', dbg[:, k * 1024:(k + 1) * 1024].bitcast(BF16), hT[:, k, :], [], 'dbg',
                           reads=[('hT', t) for t in range(4)])
            P.barrier()
        hTk = [('hT', t) for t in range(4)]
        if stage == 1:
            P.finish()
            P.emit()
            holder['nc'] = nc
            return nc


        sm = sbuf(smix, "sm", [128, 16], F32)
        junk128 = sbuf(smix, "junk128", [128, 136], F32)
        hmt = sbuf(smix, "hmt", [128, 128], BF16)
        PT = sbuf(smix, "PT", [128, 32, 128], BF16)
        psb2 = ps[2][:].bitcast(BF16)

        with ExitStack() as s2:
            wqk = sbuf(s2, "wqk", [128, KC, 512], BF16)
            wif = sbuf(s2, "wif", [128, KC, 16], BF16)
            qaT = sbuf(s2, "qaT", [128, 8, 1024], BF16)
            kaT = sbuf(s2, "kaT", [128, 4, 2048], BF16)
            gml = sbuf(s2, "gml", [128, 1024], F32)
            graw = sbuf(s2, "graw", [128, 16, 16], F32)
            tli = sbuf(s2, "tli", [128, 16, 8], F32)
            spf = sbuf(s2, "spf", [128, 16, 8], F32)
            Ecum = sbuf(s2, "Ecum", [128, 16, 8], F32)
            Bn = sbuf(s2, "Bn", [128, 16, 8], F32)
            nRn = sbuf(s2, "nRn", [128, 16, 8], F32)
            aa = sbuf(s2, "aa", [128, 16, 8], F32)
            U = sbuf(s2, "U", [128, 64, 16], F32)
            ff = sbuf(s2, "ff", [128, 8, 8], F32)
            wv = [sbuf(s2, "wv%d" % i, [128, KC, 128], BF16) for i in range(2)]
            wo = [sbuf(s2, "wo%d" % i, [128, KC, 128], BF16) for i in range(2)]
            vaug = [sbuf(s2, "vaug%d" % i, [128, 16, 136], BF16) for i in range(2)]
            gsig = [sbuf(s2, "gsig%d" % i, [128, 8, 128], F32) for i in range(2)]

            fl = lambda t: t[:].rearrange("p t h -> p (t h)")
            P.load('sp', gml[:], d_gml, ['gml'], 'gml')
            P.load('pool', wqk[:], win_v[:, :, 0:512], ['wqk'], 'wqk0')
            P.load('pool', wif[:], win_v[:, :, O_IA:O_IA + 16], ['wif'], 'wif')
            for i in range(2):
                P.memset('pool', vaug[i][:, :, 128:136], 1.0, [('vaug1', i)])

            P.mms([(ps[2][:, i * 16:(i + 1) * 16],
                    [(hT[:, k, i * 128:(i + 1) * 128], wif[:, k, :]) for k in range(KC)]) for i in range(16)],
                  hTk + ['wif'], PK(2))
            P.tt('dve', graw[:], ps[2][:, 0:256].rearrange("p (t g) -> p t g", g=16),
                 cp[:, C_BIF:C_BIF + 16].unsqueeze(1).to_broadcast([128, 16, 16]), ALU.add, PK(2) + ['cp'], ['graw'])
            P.act(tli[:], graw[:, :, 0:8], AF.Tanh, ['graw'], ['tli'], scale=1.0 / 15.0)
            P.act(spf[:], graw[:, :, 8:16], AF.Tanh, ['graw'], ['spf'], scale=1.0 / 15.0)
            P.act(spf[:], spf[:], AF.Exp, ['spf'], ['spf'], scale=-15.0)
            P.ts('dve', spf[:], spf[:], 1.0, None, ALU.add, None, ['spf'], ['spf'])
            P.act(spf[:], spf[:], AF.Ln, ['spf'], ['spf'])
            P.memset('dve', Ecum[:, 0, :], 0.0, ['Ecum'])
            for i in range(1, 16):
                P.tt('dve', Ecum[:, i, :], Ecum[:, i - 1, :], spf[:, i - 1, :], ALU.add, ['Ecum', 'spf'], ['Ecum'])
            P.mm(ps[3][:, 0:128], [(tri, fl(spf)), (ones, fl(Ecum))], ['cp', 'spf', 'Ecum'], PK(3))
            P.copy('dve', fl(Bn), ps[3][:, 0:128], PK(3), ['Bn'])
            P.mm(ps[3][:, 128:256], [(s127, fl(Bn))], ['cp', 'Bn'], PK(3))
            P.ts('dve', fl(nRn), ps[3][:, 128:256], -1.0, None, ALU.mult, None, PK(3), ['nRn'])
            P.stt('dve', fl(aa), fl(tli), 15.0, fl(Bn), ALU.mult, ALU.add, ['tli', 'Bn'], ['aa'])
            for h in range(8):
                for Tq in range(8):
                    P.act(U[:, h * 8 + Tq, :], aa[:, :, h], AF.Exp, ['aa', 'nRn'], ['U'],
                          bias=nRn[:, 7 + Tq, h:h + 1], scale=1.0)
            P.ts('dve', U[:, :, 0:8], U[:, :, 0:8], flag, None, ALU.mult, None, ['U', 'cp'], ['U'])
            P.tt('dve', fl(ff), fl(Bn)[:, 64:128], fl(nRn)[:, 56:120], ALU.add, ['Bn', 'nRn'], ['ff'])
            P.act(fl(ff), fl(ff), AF.Exp, ['ff'], ['ff'], scale=-1.0)

            cnt = 0
            for m in range(8):
                if m == 4:
                    P.load('pool', wqk[:], win_v[:, :, 512:1024], ['wqk'], 'wqk0')
                for g in ((2, 3) if m < 4 else (0, 1, 2, 3)):
                    b = 4 + cnt % 2
                    cnt += 1
                    mc = m % 4
                    P.mm(ps[b][:], [(wqk[:, k, mc * 128:(mc + 1) * 128], hT[:, k, g * 512:(g + 1) * 512])
                                    for k in range(KC)], ['wqk'] + hTk, PK(b))
                    if m < 4:
                        P.act(qaT[:, 2 * m, (g - 2) * 512:(g - 1) * 512], ps[b][:], AF.Identity, PK(b) + ['cp'], ['qaT'],
                              scale=cp[:, C_HM0:C_HM0 + 1])
                        P.act(qaT[:, 2 * m + 1, (g - 2) * 512:(g - 1) * 512], ps[b][:], AF.Identity, PK(b) + ['cp'], ['qaT'],
                              scale=cp[:, C_HM1:C_HM1 + 1])
                    else:
                        P.copy('dve', kaT[:, m - 4, g * 512:(g + 1) * 512], ps[b][:], PK(b), ['kaT'])

            import os as _os
            if stage == 2 and _os.environ.get('EXTRA1'):
                for i_ in range(int(_os.environ['EXTRA1'])):
                    P.mm(ps[7][:, 0:128], [(identb[:], identb[:])], ['identb'], PK(7))
            if stage == 2 and sub == 1:
                P.load('sp', dbg[:, 0:1024], U[:].rearrange("p a b -> p (a b)"), [], 'dbg', reads=['U'])
                P.load('sp', dbg[:, 1024:1088], fl(ff), [], 'dbg', reads=['ff'])
                P.load('sp', dbg[:, 1088:1216], fl(Bn), [], 'dbg', reads=['Bn'])
                P.load('sp', dbg[:, 1216:1344], fl(tli), [], 'dbg', reads=['tli'])
                P.load('sp', dbg[:, 2048:4096].bitcast(BF16).rearrange("p (a b) -> p a b", a=4), kaT[:, :, 1024:2048], [], 'dbg', reads=['kaT'])
                pass
                P.finish()
                P.emit()
                holder['nc'] = nc
                return nc
            gcnt = 0
            pcnt = 0
            for h in range(8 if sub >= 4 else 1):
                sl = h % 2
                P.load('pool', wv[sl][:], win_v[:, :, O_VA + h * 128:O_VA + (h + 1) * 128], [('wv', sl)], 'wv%d' % sl)
                P.load('pool', wo[sl][:], win_v[:, :, O_OA + h * 128:O_OA + (h + 1) * 128], [('wo', sl)], 'wo%d' % sl)
                for tq in range(4):
                    b = 4 + tq % 2
                    P.mms([(ps[b][:, i * 128:(i + 1) * 128],
                            [(hT[:, k, (tq * 4 + i) * 128:(tq * 4 + i + 1) * 128], wv[sl][:, k, :]) for k in range(KC)])
                           for i in range(4)], hTk + [('wv', sl)], PK(b))
                    P.copy('act' if tq % 2 == 0 else 'dve', vaug[sl][:, tq * 4:(tq + 1) * 4, 0:128],
                           ps[b][:].rearrange("p (t d) -> p t d", d=128), PK(b), [('vaug', sl)])
                for tq in range(2):
                    b = 4 + tq % 2
                    P.mms([(ps[b][:, i * 128:(i + 1) * 128],
                            [(hT[:, k, 1024 + (tq * 4 + i) * 128:1024 + (tq * 4 + i + 1) * 128], wo[sl][:, k, :])
                             for k in range(KC)]) for i in range(4)], hTk + [('wo', sl)], PK(b))
                    P.act(gsig[sl][:, tq * 4:(tq + 1) * 4, :], ps[b][:].rearrange("p (t d) -> p t d", d=128),
                          AF.Sigmoid, PK(b), [('gsig', sl)])
                P.tt('dve', gsig[sl][:], gsig[sl][:],
                     gml[:, h * 128:(h + 1) * 128].unsqueeze(1).to_broadcast([128, 8, 128]), ALU.mult,
                     [('gsig', sl), 'gml'], [('gsig', sl)])
                if stage == 2 and _os.environ.get('EXTRA2'):
                    for i_ in range(int(_os.environ['EXTRA2'])):
                        P.mm(ps[7][:, 0:128], [(identb[:], identb[:])], ['identb'], PK(7))
                if stage == 2 and sub == 2:
                    P.load('sp', dbg[:, 0:1024], gsig[sl][:].rearrange("p a b -> p (a b)"), [], 'dbg', reads=[('gsig', sl)])
                    P.finish()
                    P.emit()
                    holder['nc'] = nc
                    return nc
                hp, hm_ = h % 2, h // 2
                kq = slice(hp * 64, (hp + 1) * 64)
                for Tq in (range(8 if not (stage == 2 and 3 <= sub < 4) else int(round((sub - 3) * 10)) + 1) if sub not in (3.9, 3.05, 3.06) else ([1] if sub == 3.9 else [0])):
                    T = 8 + Tq
                    ntile = T + 1
                    nb = 5 + Tq % 2
                    if _os.environ.get('RESET'):
                        nb = 5
                        gcnt = 0
                        pcnt = 0
                    nd = ps[nb][:, 0:136]
                    pv_all = []
                    for g0 in (range(0, ntile, 4) if not (_os.environ.get('SKIP2') in ('att', 'stev') and Tq >= 1) else []):
                        Ss = list(range(g0, min(g0 + 4, ntile)))
                        b = gcnt % 2
                        gcnt += 1
                        P.mms([(ps[b][:, j * 128:(j + 1) * 128],
                                [(kaT[:, hm_, S * 128:(S + 1) * 128], qaT[:, h, Tq * 128:(Tq + 1) * 128])])
                               for j, S in enumerate(Ss)], ['kaT', 'qaT'], PK(b))
                        pv = []
                        for j, S in enumerate(Ss):
                            slot = pcnt % 32
                            pcnt += 1
                            ucol = U[:, h * 8 + Tq, S:S + 1]
                            src = ps[b][:, j * 128:(j + 1) * 128]
                            ev2 = _os.environ.get('EV2') if Tq >= 1 else None
                            if ev2 == 'none':
                                pass
                            elif ev2 == 'mix' and pcnt % 2 == 0:
                                P.act(PT[:, slot, :], src, AF.Identity, [('ps', b, j), 'U'], [('PT', slot)], scale=ucol)
                            elif ev2 == 'mix':
                                P.ts('dve', PT[:, slot, :], src, ucol, None, ALU.mult, None,
                                     [('ps', b, j), 'U'], [('PT', slot)])
                            elif ev2 == 'act':
                                P.act(PT[:, slot, :], src, AF.Identity, [('ps', b, j), 'U'], [('PT', slot)], scale=ucol)
                            elif ev2 == 'dve':
                                P.ts('dve', PT[:, slot, :], src, ucol, None, ALU.mult, None,
                                     [('ps', b, j), 'U'], [('PT', slot)])
                            else:
                                P.ts('dve', PT[:, slot, :], src, ucol, None, ALU.mult, None,
                                     [('ps', b, j), 'U'], [('PT', slot)])
                                if S == T:
                                    P.tt('dve', PT[:, slot, :], PT[:, slot, :], maskb[:], ALU.mult,
                                         [('PT', slot), 'maskb'], [('PT', slot)])
                            pv.append((slot, S))

                        if stage == 2 and sub == 2.3:
                            P.load('sp', dbg[:, 0:512].bitcast(BF16), PT[:].rearrange("p a b -> p (a b)"), [], 'dbg',
                                   reads=[('PT', i_) for i_ in range(16)])
                            P.finish()
                            P.emit()
                            holder['nc'] = nc
                            return nc

                        pv_all.extend(pv)

                    def pvfn(e, pv=list(pv_all), nd=nd, sl=sl, ntile=ntile):
                        ins = None
                        for slot, S in pv:
                            ins = e.matmul(nd, lhsT=PT[:, slot, :], rhs=vaug[sl][:, S, :],
                                           start=(S == 0), stop=(S == ntile - 1))
                        return ins
                    if _os.environ.get('SKIP2') == 'stev' and Tq >= 1:
                        pv_all = [(S_, S_) for S_ in range(9)]

                        def pvfn(e, pv=list(pv_all), nd=nd, sl=sl, ntile=9):
                            ins = None
                            for slot, S in pv:
                                ins = e.matmul(nd, lhsT=PT[:, slot, :], rhs=vaug[sl][:, S, :], start=(S == 0), stop=(S == ntile - 1))
                            return ins
                    if pv_all and not (_os.environ.get('SKIP2') == 'pv' and Tq >= 1):
                        P.op('pe', pvfn, [('PT', s_) for s_, _ in pv_all] + [('vaug', sl), ('vaug1', sl)], PK(nb))
                    if stage == 2 and _os.environ.get('EXTRA3'):
                        for i_ in range(int(_os.environ['EXTRA3'])):
                            P.mm(ps[7][:, 0:128], [(identb[:], identb[:])], ['identb'], PK(7))
                    if stage == 2 and sub == 2.5:
                        P.copy('dve', junk128[:, 0:130], nd[:, 0:130], PK(nb), ['junk128'])
                        P.load('sp', dbg[:, 0:130], junk128[:, 0:130], [], 'dbg', reads=['junk128'])
                        P.finish()
                        P.emit()
                        holder['nc'] = nc
                        return nc
                    if _os.environ.get('SKIP2') in ('epi', 'pv', 'stev') and Tq >= 1:
                        continue
                    fcol = ff[:, Tq, h:h + 1]
                    P.tt('dve', sm[:, 0:1], nd[:, 128:129], fcol, ALU.mult, PK(nb) + ['ff'], ['sm'])
                    P.stt('dve', sm[:, 1:2], sm[:, 0:1], -1.0, sm[:, 0:1], ALU.mult, ALU.max, ['sm'], ['sm'])
                    P.ts('dve', sm[:, 1:2], sm[:, 1:2], 1.0, None, ALU.max, None, ['sm'], ['sm'])
                    P.recip(sm[:, 2:3], sm[:, 1:2], ['sm'], ['sm'])
                    P.tt('dve', sm[:, 3:4], sm[:, 2:3], fcol, ALU.mult, ['sm', 'ff'], ['sm'])
                    if stage == 2 and _os.environ.get('EXTRA6'):
                        for i_ in range(int(_os.environ['EXTRA6'])):
                            P.mm(ps[7][:, 0:128], [(identb[:], identb[:])], ['identb'], PK(7))
                    if stage == 2 and sub == 2.6:
                        P.load('sp', dbg[:, 0:16], sm[:], [], 'dbg', reads=['sm'])
                        P.finish()
                        P.emit()
                        holder['nc'] = nc
                        return nc
                    P.act(junk128[:, 0:128], nd[:, 0:128], AF.Square, PK(nb) + ['sm'], ['junk128', 'sm'],
                          scale=sm[:, 3:4], accum_out=sm[:, 4:5])
                    rstd_from_ssq(sm[:, 4:5], sm[:, 5:6], 1, 1.0 / 128, 'sm', 'sm')
                    P.tt('dve', sm[:, 6:7], sm[:, 5:6], sm[:, 3:4], ALU.mult, ['sm'], ['sm'])
                    P.stt('dve', hmt[:], nd[:, 0:128], sm[:, 6:7], gsig[sl][:, Tq, :], ALU.mult, ALU.mult,
                          PK(nb) + ['sm', ('gsig', sl)], ['hmt'])
                    if stage == 2 and _os.environ.get('EXTRA7'):
                        for i_ in range(int(_os.environ['EXTRA7'])):
                            P.mm(ps[7][:, 0:128], [(identb[:], identb[:])], ['identb'], PK(7))
                    if stage == 2 and sub == 2.7:
                        P.load('sp', dbg[:, 0:64].bitcast(BF16), hmt[:], [], 'dbg', reads=['hmt'])
                        P.finish()
                        P.emit()
                        holder['nc'] = nc
                        return nc
                    P.mm(ps[2][:, 0:128], [(hmt[:], identb[:])], ['hmt', 'identb'], PK(2))
                    P.copy('act', hmT[:, h, Tq * 128:(Tq + 1) * 128], ps[2][:, 0:128], PK(2), ['hmT'])
                    P.barrier()
            if stage == 2 and _os.environ.get('EXTRA9'):
                n_, l_, b_ = _os.environ['EXTRA9'].split(',')
                for i_ in range(int(n_)):
                    P.mm(ps[int(b_)][:, 0:128], [((hmt if l_ == 'hmt' else identb)[:], identb[:])], ['hmt', 'identb'], PK(int(b_)))
            if stage == 2 and _os.environ.get('EXTRA8'):
                for i_ in range(int(_os.environ['EXTRA8'])):
                    P.mm(ps[7][:, 0:128], [(identb[:], identb[:])], ['identb'], PK(7))
            if stage == 2 and sub == 3.06:
                for _ in range(40):
                    P.mm(ps[3][:, 0:128], [(hmt[:], identb[:])], ['hmt', 'identb'], PK(3))
            if stage == 2 and sub == 3.05:
                for _ in range(30):
                    P.act(small[:, 32:40], small[:, 40:48], AF.Identity, ['smallx'], ['smallx'], scale=1.0)
            if stage == 2:
                nq_ = 8 if not (3 <= sub < 4) else (int(round((sub - 3) * 10)) + 1 if sub != 3.9 else 2)
                if sub in (3.05, 3.06):
                    nq_ = 1
                for h in range(8 if sub >= 4 else 1):
                    P.load('sp', dbg[:, h * 512:h * 512 + nq_ * 64].bitcast(BF16), hmT[:, h, 0:nq_ * 128], [], 'dbg', reads=['hmT'])
            P.barrier()
        if stage == 2:
            P.finish()
            P.emit()
            holder['nc'] = nc
            return nc
        obT = sbuf(smix, "obT", [128, 8, 1024], BF16)


        with ExitStack() as s3:
            wqb = [sbuf(s3, "wqb%d" % i, [128, KC, 128], BF16) for i in range(2)]
            wkb = [sbuf(s3, "wkb%d" % i, [128, KC, 128], BF16) for i in range(2)]
            wvb = [sbuf(s3, "wvb%d" % i, [128, KC, 128], BF16) for i in range(2)]
            qbT = sbuf(s3, "qbT", [128, 1024], BF16)
            kbT = sbuf(s3, "kbT", [128, 2048], BF16)
            vb = sbuf(s3, "vb", [128, 16, 136], BF16)
            sqj = sbuf(s3, "sqj", [128, 512], F32)
            qn = sbuf(s3, "qn", [128, 4, 128], F32)
            ssq4 = sbuf(s3, "ssq4", [128, 4], F32)
            rs4 = sbuf(s3, "rs4", [128, 4], F32)
            ksum = sbuf(s3, "ksum", [128, 8], F32)
            kmr = sbuf(s3, "kmr", [128, 8], F32)
            kmh = sbuf(s3, "kmh", [128, 8], BF16)
            kml = sbuf(s3, "kml", [128, 8], BF16)
            gm = sbuf(s3, "gm", [128, 64], F32)
            top8 = sbuf(s3, "top8", [128, 8, 8], F32)
            sel = sbuf(s3, "sel", [128, 64], F32)
            acc = sbuf(s3, "acc", [128, 136], F32)
            rden = sbuf(s3, "rden", [128, 1], F32)
            P.memset('pool', vb[:, :, 128:136], 1.0, ['vb1'])

            def norm_T(b, dst, gcol, dkey):
                P.act(sqj[:], ps[b][:], AF.Square, PK(b), ['sqj'])
                P.reduce(ssq4[:], sqj[:].rearrange("p (t d) -> p t d", d=128), ALU.add, ['sqj'], ['ssq4'])
                rstd_from_ssq(ssq4[:], rs4[:], 4, 1.0 / 128, 'ssq4', 'rs4')
                for i in range(4):
                    P.ts('dve', qn[:, i, :], ps[b][:, i * 128:(i + 1) * 128], rs4[:, i:i + 1], None, ALU.mult, None,
                         PK(b) + ['rs4'], [('qn', i)])
                P.transposes([(ps[2][:, i * 128:(i + 1) * 128], qn[:, i, :]) for i in range(4)], ident,
                             [('qn', i) for i in range(4)] + ['cp'], PK(2))
                P.act(dst, ps[2][:], AF.Identity, PK(2) + ['cp', 'gqs'], [dkey], scale=gcol)

            gcnt = 0
            rcnt = 0
            for h in range(8):
                sl = h % 2
                P.load('pool', wqb[sl][:], win_v[:, :, O_QB + h * 128:O_QB + (h + 1) * 128], [('wqb', sl)], 'wqb%d' % sl)
                P.load('pool', wkb[sl][:], win_v[:, :, O_KB + h * 128:O_KB + (h + 1) * 128], [('wkb', sl)], 'wkb%d' % sl)
                P.load('pool', wvb[sl][:], win_v[:, :, O_VB + h * 128:O_VB + (h + 1) * 128], [('wvb', sl)], 'wvb%d' % sl)
                for tq in range(2):
                    b = 4 + tq % 2
                    P.mms([(ps[b][:, i * 128:(i + 1) * 128],
                            [(hT[:, k, 1024 + (tq * 4 + i) * 128:1024 + (tq * 4 + i + 1) * 128], wqb[sl][:, k, :])
                             for k in range(KC)]) for i in range(4)], hTk + [('wqb', sl)], PK(b))
                    norm_T(b, qbT[:, tq * 512:(tq + 1) * 512], gqs[:, 0:1], 'qbT')
                for tq in range(4):
                    b = 4 + tq % 2
                    P.mms([(ps[b][:, i * 128:(i + 1) * 128],
                            [(hT[:, k, (tq * 4 + i) * 128:(tq * 4 + i + 1) * 128], wkb[sl][:, k, :])
                             for k in range(KC)]) for i in range(4)], hTk + [('wkb', sl)], PK(b))
                    norm_T(b, kbT[:, tq * 512:(tq + 1) * 512], cp[:, C_GK:C_GK + 1], 'kbT')
                for tq in range(4):
                    b = 4 + tq % 2
                    P.mms([(ps[b][:, i * 128:(i + 1) * 128],
                            [(hT[:, k, (tq * 4 + i) * 128:(tq * 4 + i + 1) * 128], wvb[sl][:, k, :])
                             for k in range(KC)]) for i in range(4)], hTk + [('wvb', sl)], PK(b))
                    P.copy('dve', vb[:, tq * 4:(tq + 1) * 4, 0:128],
                           ps[b][:].rearrange("p (t d) -> p t d", d=128), PK(b), ['vb'])
                P.reduce(ksum[:], kbT[:].rearrange("p (n k) -> p n k", k=256), ALU.add, ['kbT'], ['ksum'])
                P.ts('dve', kmr[:], ksum[:], 1.0 / 256, None, ALU.mult, None, ['ksum'], ['kmr'])
                P.copy('dve', kmh[:], kmr[:], ['kmr'], ['kmh'])
                P.tt('dve', kml[:], kmr[:], kmh[:], ALU.subtract, ['kmr', 'kmh'], ['kml'])
                P.mms([(ps[3][:, Tq * 8:(Tq + 1) * 8],
                        [(qbT[:, Tq * 128:(Tq + 1) * 128], kmh[:]), (qbT[:, Tq * 128:(Tq + 1) * 128], kml[:])])
                       for Tq in range(8)], ['qbT', 'kmh', 'kml'], PK(3))
                P.tt('dve', gm[:], ps[3][:, 0:64], cp[:, C_GMASK:C_GMASK + 64], ALU.add, PK(3) + ['cp'], ['gm'])
                for Tq in range(8):
                    P.op('dve', lambda e, Tq=Tq: e.max(out=top8[:, Tq, :], in_=gm[:, Tq * 8:(Tq + 1) * 8]),
                         ['gm'], [('top8', Tq)])
                    P.ts('dve', sel[:, Tq * 8:(Tq + 1) * 8], gm[:, Tq * 8:(Tq + 1) * 8], top8[:, Tq, 2:3], None,
                         ALU.is_ge, None, ['gm', ('top8', Tq)], ['sel'])
                P.tt('dve', sel[:], sel[:], cp[:, C_VALID:C_VALID + 64], ALU.mult, ['sel', 'cp'], ['sel'])
                for Tq in range(8):
                    T = 8 + Tq
                    ntile = T + 1
                    for g0 in range(0, ntile, 4):
                        Ss = list(range(g0, min(g0 + 4, ntile)))
                        nj = len(Ss)
                        b = gcnt % 2
                        gcnt += 1
                        P.mms([(ps[b][:, j * 128:(j + 1) * 128],
                                [(kbT[:, S * 128:(S + 1) * 128], qbT[:, Tq * 128:(Tq + 1) * 128])])
                               for j, S in enumerate(Ss)], ['kbT', 'qbT'], PK(b))
                        P.act(PT[:, g0:g0 + nj, :], ps[b][:, 0:nj * 128].rearrange("p (t d) -> p t d", d=128),
                              AF.Exp, PK(b), [('PT', S) for S in Ss])
                    P.tt('dve', PT[:, T, :], PT[:, T, :], maskb[:], ALU.mult, [('PT', T), 'maskb'], [('PT', T)])
                    nown = T // 2
                    for n in range(nown + 1):
                        tiles = [S for S in (2 * n, 2 * n + 1) if S <= T]
                        r = rcnt % 4
                        rcnt += 1
                        reg = ps[6 + r % 2][:, (r // 2) * 136:(r // 2) * 136 + 136]
                        P.mm(reg, [(PT[:, S, :], vb[:, S, :]) for S in tiles],
                             [('PT', S) for S in tiles] + ['vb', 'vb1'], [('pvr', r)])
                        if n == nown:
                            P.tt('dve', acc[:], reg, acc[:], ALU.add, [('pvr', r), 'acc'], ['acc'])
                        elif n == 0:
                            P.ts('dve', acc[:], reg, sel[:, Tq * 8 + n:Tq * 8 + n + 1], None, ALU.mult, None,
                                 [('pvr', r), 'sel'], ['acc'])
                        else:
                            P.stt('dve', acc[:], reg, sel[:, Tq * 8 + n:Tq * 8 + n + 1], acc[:], ALU.mult, ALU.add,
                                  [('pvr', r), 'sel', 'acc'], ['acc'])
                    P.recip(rden[:], acc[:, 128:129], ['acc'], ['rden'])
                    P.ts('dve', hmt[:], acc[:, 0:128], rden[:, 0:1], None, ALU.mult, None, ['acc', 'rden'], ['hmt'])
                    P.mm(ps[2][:, 0:128], [(hmt[:], identb[:])], ['hmt', 'identb'], PK(2))
                    P.copy('act', obT[:, h, Tq * 128:(Tq + 1) * 128], ps[2][:, 0:128], PK(2), ['obT'])
            if stage == 3:
                for h in range(8):
                    P.load('sp', dbg[:, h * 512:(h + 1) * 512].bitcast(BF16), obT[:, h, :], [], 'dbg', reads=['obT'])
            P.barrier()
        if stage == 3:
            P.finish()
            P.emit()
            holder['nc'] = nc
            return nc


        yT = sbuf(smix, "yT", [128, KC, 1024], BF16)
        wa_v = d_wa.rearrange("(k p) n -> p k n", p=128)
        wb_v = d_wb.rearrange("(k p) n -> p k n", p=128)
        with ExitStack() as s4:
            wga = sbuf(s4, "wga", [128, KC, 256], BF16)
            wgb = sbuf(s4, "wgb", [128, KC, 256], BF16)
            wa4 = sbuf(s4, "wa4", [128, 8, 256], BF16)
            wb4 = sbuf(s4, "wb4", [128, 8, 256], BF16)
            sga = sbuf(s4, "sga", [128, 512], F32)
            sgb = sbuf(s4, "sgb", [128, 512], F32)
            t1 = sbuf(s4, "t1", [128, 512], F32)
            cnt = 0
            for cg in range(8):
                P.load('pool', wga[:], win_v[:, :, O_GA + cg * 256:O_GA + (cg + 1) * 256], ['wga'], 'wga')
                P.load('pool', wgb[:], win_v[:, :, O_GB + cg * 256:O_GB + (cg + 1) * 256], ['wgb'], 'wgb')
                P.load('pool', wa4[:], wa_v[:, :, cg * 256:(cg + 1) * 256], ['wa4'], 'wa4')
                P.load('pool', wb4[:], wb_v[:, :, cg * 256:(cg + 1) * 256], ['wb4'], 'wb4')
                for jj in range(2):
                    j = cg * 2 + jj
                    cs = slice(jj * 128, (jj + 1) * 128)
                    for g in range(2):
                        bga, bgb, bya, byb = (0, 1, 4, 5) if cnt % 2 == 0 else (2, 3, 6, 7)
                        cnt += 1
                        ts_ = slice(g * 512, (g + 1) * 512)
                        to_ = slice(1024 + g * 512, 1024 + (g + 1) * 512)
                        P.mm(ps[bga][:], [(wga[:, k, cs], hT[:, k, to_]) for k in range(KC)], ['wga'] + hTk, PK(bga))
                        P.mm(ps[bgb][:], [(wgb[:, k, cs], hT[:, k, to_]) for k in range(KC)], ['wgb'] + hTk, PK(bgb))
                        P.mm(ps[bya][:], [(wa4[:, k, cs], hmT[:, k, ts_]) for k in range(8)], ['wa4', 'hmT'], PK(bya))
                        P.mm(ps[byb][:], [(wb4[:, k, cs], obT[:, k, ts_]) for k in range(8)], ['wb4', 'obT'], PK(byb))
                        P.act(sga[:], ps[bga][:], AF.Sigmoid, PK(bga), ['sga'])
                        P.act(sgb[:], ps[bgb][:], AF.Sigmoid, PK(bgb), ['sgb'])
                        P.tt('dve', t1[:], sga[:], ps[bya][:], ALU.mult, ['sga'] + PK(bya), ['t1'])
                        P.tt('dve', sgb[:], sgb[:], ps[byb][:], ALU.mult, ['sgb'] + PK(byb), ['sgb'])
                        P.tt('dve', yT[:, j, ts_], t1[:], sgb[:], ALU.add, ['t1', 'sgb'], ['yT'])
            P.barrier()

        wout_v = d_wout.rearrange("(k p) n -> p k n", p=128)

        def x1k(tile, c0, c1):
            return [('x1', tile, c) for c in range(c0, c1)]
        with ExitStack() as s5:
            wo4 = [sbuf(s5, "wo4%d" % i, [128, KC, 512], BF16) for i in range(2)]
            xr = [sbuf(s5, "xr%d" % i, [128, 512], F32) for i in range(2)]
            tmpo = sbuf(s5, "tmpo", [128, 512], F32)
            for cg in range(4):
                sl = cg % 2
                P.load('pool', wo4[sl][:], wout_v[:, :, cg * 512:(cg + 1) * 512], [('wo4', sl)], 'wo4%d' % sl)
                for tile in range(8):
                    b = tile % 2
                    xs_ = tile % 2
                    P.load('sp', xr[xs_][:], d_xown[tile * 128:(tile + 1) * 128, cg * 512:(cg + 1) * 512],
                           [('xr', xs_)], 'xr%d' % xs_)
                    P.mm(ps[b][:], [(yT[:, k, tile * 128:(tile + 1) * 128], wo4[sl][:, k, :]) for k in range(KC)],
                         ['yT', ('wo4', sl)], PK(b))
                    P.tt('dve', tmpo[:], ps[b][:], gt1[:, cg * 512:(cg + 1) * 512], ALU.mult, PK(b) + gt1k, ['tmpo'])
                    P.tt('dve', x1[:, tile, cg * 512:(cg + 1) * 512], tmpo[:], xr[xs_][:], ALU.add,
                         ['tmpo', ('xr', xs_)], x1k(tile, cg * 2, cg * 2 + 2))
            if stage == 4:
                for tile in range(8):
                    P.load('sp', dbg[:, tile * 2048:(tile + 1) * 2048], x1[:, tile, :], [], 'dbg',
                           reads=x1k(tile, 0, 8))
            P.barrier()
        if stage == 4:
            P.finish()
            P.emit()
            holder['nc'] = nc
            return nc
        try:
            smix.close()
        except AssertionError:
            pass
        P.barrier()

        with ExitStack() as s6:
            tT = sbuf(s6, "tT", [128, KC, 1024], BF16)
            actT = sbuf(s6, "actT", [128, KC, 1024], BF16)
            with ExitStack() as s6a:
                xs = sbuf(s6a, "xs", [128, D], F32)
                wr = sbuf(s6a, "wr", [128, KC, NEXP], BF16)
                lg = sbuf(s6a, "lg", [128, 8, NEXP], F32)
                ex = sbuf(s6a, "ex", [128, NEXP], F32)
                mk = sbuf(s6a, "mk", [128, NEXP], F32)
                t8 = sbuf(s6a, "t8", [128, 8, 8], F32)
                ssqm = sbuf(s6a, "ssqm", [128, 8], F32)
                rstm = sbuf(s6a, "rstm", [128, 8], F32)
                sm2 = sbuf(s6a, "sm2", [128, 8], F32)
                combT = sbuf(s6a, "combT", [32, 8, 128], F32)
                bdn = sbuf(s6a, "bdn", [32, D], F32)
                tmpb = sbuf(s6a, "tmpb", [128, 512], F32)
                P.load('pool', wr[:], d_wr.rearrange("(k p) n -> p k n", p=128), ['wr'], 'wr')
                P.load('sp', bdn[:], d_bdn, ['bdn'], 'bdn')
                for tile in range(8):
                    P.act(xs[:], x1[:, tile, :], AF.Square, x1k(tile, 0, 8), ['xs', ('ssqm', tile)],
                          accum_out=ssqm[:, tile:tile + 1])
                    rstd_from_ssq(ssqm[:, tile:tile + 1], rstm[:, tile:tile + 1], 1, 1.0 / D, ('ssqm', tile), ('rstm', tile))
                    P.ts('dve', xs[:], x1[:, tile, :], rstm[:, tile:tile + 1], None, ALU.mult, None,
                         x1k(tile, 0, 8) + [('rstm', tile)], ['xs'])
                    for kg in range(4):
                        b = kg % 2
                        P.transposes([(ps[b][:, i * 128:(i + 1) * 128], xs[:, (kg * 4 + i) * 128:(kg * 4 + i + 1) * 128])
                                      for i in range(4)], ident, ['xs', 'cp'], PK(b))
                        for i in range(4):
                            k = kg * 4 + i
                            P.act(tT[:, k, tile * 128:(tile + 1) * 128], ps[b][:, i * 128:(i + 1) * 128], AF.Identity,
                                  [('ps', b, i), 's2c', 'modc'], ['tT'], scale=s2c[:, k:k + 1], bias=modc[:, 48 + k:49 + k])
                P.mms([(ps[2][:, tile * 32:(tile + 1) * 32],
                        [(tT[:, k, tile * 128:(tile + 1) * 128], wr[:, k, :]) for k in range(KC)]) for tile in range(8)],
                      ['tT', 'wr'], PK(2))
                P.tt('dve', lg[:], ps[2][:, 0:256].rearrange("p (t e) -> p t e", e=NEXP),
                     cp[:, C_BROUT:C_BROUT + NEXP].unsqueeze(1).to_broadcast([128, 8, NEXP]), ALU.add,
                     PK(2) + ['cp'], ['lg'])
                for tile in range(8):
                    P.op('dve', lambda e, tile=tile: e.max(out=t8[:, tile, :], in_=lg[:, tile, :]), ['lg'], [('t8', tile)])
                    P.ts('dve', sm2[:, 0:1], t8[:, tile, 0:1], -1.0, None, ALU.mult, None, [('t8', tile)], ['sm2'])
                    P.act(ex[:], lg[:, tile, :], AF.Exp, ['lg', 'sm2'], ['ex'], bias=sm2[:, 0:1], scale=1.0)
                    P.ts('dve', mk[:], lg[:, tile, :], t8[:, tile, 3:4], None, ALU.is_ge, None, ['lg', ('t8', tile)], ['mk'])
                    P.tt('dve', ex[:], ex[:], mk[:], ALU.mult, ['ex', 'mk'], ['ex'])
                    P.reduce(sm2[:, 1:2], ex[:], ALU.add, ['ex'], ['sm2'])
                    P.recip(sm2[:, 2:3], sm2[:, 1:2], ['sm2'], ['sm2'])
                    P.ts('dve', comb[:, tile, :], ex[:], sm2[:, 2:3], None, ALU.mult, None, ['ex', 'sm2'], [('comb', tile)])
                combk = [('comb', t_) for t_ in range(8)]
                for half in range(2):
                    P.transposes([(ps[3 + half][0:32, i * 128:(i + 1) * 128], comb[:, half * 4 + i, :]) for i in range(4)],
                                 ident, combk + ['cp'], PK(3 + half))
                    P.copy('dve', combT[:, half * 4:(half + 1) * 4, :],
                           ps[3 + half][0:32, :].rearrange("p (t d) -> p t d", d=128), PK(3 + half), ['combT'])
                for tile in range(8):
                    for cg in range(4):
                        b = 5 + (tile * 4 + cg) % 2
                        P.mm(ps[b][:], [(combT[:, tile, :], bdn[:, cg * 512:(cg + 1) * 512])], ['combT', 'bdn'], PK(b))
                        P.tt('dve', tmpb[:], ps[b][:], gt2[:, cg * 512:(cg + 1) * 512], ALU.mult, PK(b) + gt2k, ['tmpb'])
                        P.tt('dve', x1[:, tile, cg * 512:(cg + 1) * 512], tmpb[:], x1[:, tile, cg * 512:(cg + 1) * 512],
                             ALU.add, ['tmpb'] + x1k(tile, cg * 2, cg * 2 + 2), x1k(tile, cg * 2, cg * 2 + 2))
                P.barrier()
            with ExitStack() as s6b:
                wupg = [sbuf(s6b, "wupg%d" % i, [128, KC, 256], BF16) for i in range(2)]
                wupl = [sbuf(s6b, "wupl%d" % i, [128, KC, 256], BF16) for i in range(2)]
                wdn = [sbuf(s6b, "wdn%d" % i, [128, KC, 256], BF16) for i in range(2)]
                bup = sbuf(s6b, "bup", [128, NEXP * 32], F32)
                g_ = sbuf(s6b, "g_", [128, 512], F32)
                sg = sbuf(s6b, "sg", [128, 512], F32)
                l_ = sbuf(s6b, "l_", [128, 512], F32)
                tmpd = l_[:, 0:256]
                P.load('sp', bup[:], d_bup, ['bup'], 'bup')
                cnt = 0
                scnt = 0
                dcnt = 0
                for e_ in range(NEXP):
                    wup_e = d_wup[e_].rearrange("(k p) n -> p k n", p=128)
                    wdn_e = d_wdn[e_].rearrange("(k p) n -> p k n", p=128)
                    for mp2 in range(8):
                        st = scnt % 2
                        scnt += 1
                        P.load('pool', wupg[st][:], wup_e[:, :, mp2 * 256:(mp2 + 1) * 256], [('wupg', st)], 'wupg%d' % st)
                        P.load('pool', wupl[st][:], wup_e[:, :, D + mp2 * 256:D + (mp2 + 1) * 256], [('wupl', st)],
                               'wupl%d' % st)
                        for mi in range(2):
                            mp = mp2 * 2 + mi
                            cs = slice(mi * 128, (mi + 1) * 128)
                            for g in range(2):
                                bA, bB = (0, 1) if cnt % 2 == 0 else (2, 3)
                                cnt += 1
                                ts_ = slice(g * 512, (g + 1) * 512)
                                P.mm(ps[bA][:], [(wupg[st][:, k, cs], tT[:, k, ts_]) for k in range(KC)],
                                     [('wupg', st), 'tT'], PK(bA))
                                P.mm(ps[bB][:], [(wupl[st][:, k, cs], tT[:, k, ts_]) for k in range(KC)],
                                     [('wupl', st), 'tT'], PK(bB))
                                P.ts('dve', g_[:], ps[bA][:], bup[:, e_ * 32 + mp:e_ * 32 + mp + 1], 7.0, ALU.add, ALU.min,
                                     PK(bA) + ['bup'], ['g_'])
                                P.act(sg[:], g_[:], AF.Sigmoid, ['g_'], ['sg'], scale=1.702)
                                P.ts('dve', l_[:], ps[bB][:], bup[:, e_ * 32 + 16 + mp:e_ * 32 + 17 + mp], -7.0,
                                     ALU.add, ALU.max, PK(bB) + ['bup'], ['l_'])
                                P.ts('dve', l_[:], l_[:], 7.0, 1.0, ALU.min, ALU.add, ['l_'], ['l_'])
                                P.tt('dve', g_[:], g_[:], sg[:], ALU.mult, ['g_', 'sg'], ['g_'])
                                P.tt('dve', actT[:, mp, ts_], g_[:], l_[:], ALU.mult, ['g_', 'l_'], ['actT'])
                    for cgd in range(8):
                        ds_ = dcnt % 2
                        dcnt += 1
                        P.load('pool', wdn[ds_][:], wdn_e[:, :, cgd * 256:(cgd + 1) * 256], [('wdn', ds_)], 'wdn%d' % ds_)
                        for tile in range(8):
                            b = 4 + tile % 2
                            P.mm(ps[b][:, 0:256], [(actT[:, k, tile * 128:(tile + 1) * 128], wdn[ds_][:, k, :])
                                                   for k in range(KC)], ['actT', ('wdn', ds_)], PK(b))
                            P.tt('dve', tmpd, ps[b][:, 0:256], gt2[:, cgd * 256:(cgd + 1) * 256], ALU.mult,
                                 PK(b) + gt2k, ['l_'])
                            P.stt('dve', x1[:, tile, cgd * 256:(cgd + 1) * 256], tmpd, comb[:, tile, e_:e_ + 1],
                                  x1[:, tile, cgd * 256:(cgd + 1) * 256], ALU.mult, ALU.add,
                                  ['l_', ('comb', tile)] + x1k(tile, cgd, cgd + 1), x1k(tile, cgd, cgd + 1))
                for tile in range(8):
                    P.load('sp', d_out[tile * 128:(tile + 1) * 128, :], x1[:, tile, :], [], 'out%d' % (tile % 2),
                           reads=x1k(tile, 0, 8))
        P.finish()
        P.emit()
        holder['nc'] = nc
        return nc


def _consts():
    cp = np.zeros((128, C_TOT), np.float32)
    cp[:, C_ID:C_ID + 128] = np.eye(128, dtype=np.float32)
    cp[:, C_TRI:C_TRI + 128] = np.triu(np.ones((128, 128), np.float32))
    cp[:, C_ONES:C_ONES + 128] = 1.0
    cp[127, C_S127:C_S127 + 128] = 1.0
    cp[:, C_MASK:C_MASK + 128] = np.triu(np.ones((128, 128), np.float32))
    cp[:64, C_HM0] = 0.125
    cp[64:, C_HM1] = 0.125
    return cp


def _prep_inputs(inputs):
    x = np.asarray(inputs["x"], np.float32)
    c = np.asarray(inputs["c"], np.float32)
    L = 0
    cp0 = _consts()
    shared = {
        "gml_bc": np.ascontiguousarray(np.broadcast_to(
            np.asarray(inputs["g_mlstm_out"], np.float32)[L].reshape(1, 1024), (128, 1024))),
        "w_ada": np.ascontiguousarray(inputs["w_ada"][L], np.float32),
        "b_ada": np.ascontiguousarray(inputs["b_ada"][L].reshape(1, -1), np.float32),
        "w_in": np.ascontiguousarray(inputs["w_in"][L], np.float32),
        "w_branch_a": np.ascontiguousarray(inputs["w_branch_a"][L], np.float32),
        "w_branch_b": np.ascontiguousarray(inputs["w_branch_b"][L], np.float32),
        "w_out": np.ascontiguousarray(inputs["w_out"][L], np.float32),
        "w_router": np.ascontiguousarray(inputs["w_router"][L], np.float32),
        "w_up": np.ascontiguousarray(inputs["w_up"][L], np.float32),
        "b_up_col": np.ascontiguousarray(
            np.asarray(inputs["b_up"], np.float32)[L].reshape(NEXP, 32, 128).transpose(2, 0, 1).reshape(128, NEXP * 32)),
        "w_down": np.ascontiguousarray(inputs["w_down"][L], np.float32),
        "b_down": np.ascontiguousarray(inputs["b_down"][L], np.float32),
    }
    col = lambda v: np.asarray(v, np.float32).reshape(16, 128).T
    in_maps = []
    for core in range(8):
        b, half = core // 2, core % 2
        cp = cp0.copy()
        cp[:, C_CCOL:C_CCOL + 16] = col(c[b])
        cp[:, C_GMIX:C_GMIX + 16] = col(inputs["g_mix"][L])
        cp[:, C_GFFN:C_GFFN + 16] = col(inputs["g_ffn"][L])
        cp[:, C_BIF:C_BIF + 8] = np.asarray(inputs["b_igate"], np.float32)[L][None, :]
        cp[:, C_BIF + 8:C_BIF + 16] = np.asarray(inputs["b_fgate"], np.float32)[L][None, :]
        cp[:, C_GQ] = np.asarray(inputs["g_q"], np.float32)[L]
        cp[:, C_GK] = np.asarray(inputs["g_k"], np.float32)[L]
        cp[:, C_FLAG] = float(half)
        valid = np.zeros((8, 8), np.float32)
        for tq in range(8):
            for n in range(8):
                if n < 4:
                    valid[tq, n] = float(half)
                else:
                    valid[tq, n] = 1.0 if (n - 4) < tq // 2 else 0.0
        cp[:, C_VALID:C_VALID + 64] = valid.reshape(1, 64)
        cp[:, C_GMASK:C_GMASK + 64] = np.where(valid.reshape(1, 64) > 0, 0.0, -1e30)
        cp[:, C_BROUT:C_BROUT + 32] = np.asarray(inputs["b_router"], np.float32)[L][None, :]
        m = dict(shared)
        m["cpack"] = cp
        m["x_own"] = np.ascontiguousarray(x[b, half * 1024:(half + 1) * 1024])
        m["x_ctx"] = np.ascontiguousarray(x[b, 0:1024]) if half == 1 else np.zeros((1024, D), np.float32)
        in_maps.append(m)
    return in_maps


def kernel(**inputs):
    in_maps = _prep_inputs(inputs)
    nc = build_program()
    res = run_bass_kernel_spmd(nc, in_maps, core_ids=list(range(8)))
    out = np.zeros((4, 2048, D), np.float32)
    for core in range(8):
        b, half = core // 2, core % 2
        out[b, half * 1024:(half + 1) * 1024] = res.results[core]["out"]
    return out
```
